# Optimizing a Trainium2 kernel written in Bass

```python
import jax, jax.numpy as jnp
from jax import lax
import numpy as np

D_MODEL = 2048
BATCH = 4
SEQ = 2048
DEPTH = 4

CHUNK = 64
EPS = 1e-6
CONV_W = 3
GLA_HEADS = 4
GLA_DK = D_MODEL // 2
GLA_DV = D_MODEL
GLA_DK_HEAD = GLA_DK // GLA_HEADS
GLA_DV_HEAD = GLA_DV // GLA_HEADS
GLA_GATE_RANK = 16
GLA_GATE_NORM = 16.0
GLA_IN = 2 * GLA_DK + 2 * GLA_DV + GLA_GATE_RANK
D_FF = ((8 * D_MODEL // 3 + 255) // 256) * 256
N_EXPERTS = 8
TOP_K = 2
D_FF_EXPERT = D_FF
MOE_BLOCK = 256
N_CONV_LAYERS = (DEPTH + 1) // 2
N_GLA_LAYERS = DEPTH // 2

kernel_name = "hybrid_conv_gla_moe_adaln_trunk"


def _rmsnorm(x, g):
    x32 = x.astype(jnp.float32)
    y = x32 * lax.rsqrt(jnp.mean(x32 * x32, axis=-1, keepdims=True) + EPS) * g.astype(jnp.float32)
    return y.astype(x.dtype)


def _modulate(h, shift, scale):
    return h * (1 + scale) + shift


def _swiglu(h, w13, w2):
    a, b = jnp.split(h @ w13, 2, axis=-1)
    return (jax.nn.silu(a) * b) @ w2


def _conv_mixer(h, w_in, k_conv, w_out):
    S_ = h.shape[1]
    gb, gc, u = jnp.split(h @ w_in, 3, axis=-1)
    v = gc * u
    vp = jnp.pad(v, ((0, 0), (CONV_W - 1, 0), (0, 0)))
    conv = k_conv[0] * vp[:, 0:S_]
    for i in range(1, CONV_W):
        conv = conv + k_conv[i] * vp[:, i:i + S_]
    return (gb * conv) @ w_out


def _gla_mixer(h, w_in, w_gk, b_gk, norm_g, w_out):
    B_, S_, _ = h.shape
    NC = S_ // CHUNK
    proj = h @ w_in
    q, k, v, g, gk_low = jnp.split(
        proj, [GLA_DK, 2 * GLA_DK, 2 * GLA_DK + GLA_DV, 2 * GLA_DK + 2 * GLA_DV], axis=-1)
    log_a = jax.nn.log_sigmoid((gk_low @ w_gk + b_gk).astype(jnp.float32)) / GLA_GATE_NORM

    def to_chunks(t, dh):
        return t.astype(jnp.float32).reshape(B_, NC, CHUNK, GLA_HEADS, dh).transpose(1, 0, 3, 2, 4)

    qc = to_chunks(q, GLA_DK_HEAD) * (GLA_DK_HEAD ** -0.5)
    kc = to_chunks(k, GLA_DK_HEAD)
    vc = to_chunks(v, GLA_DV_HEAD)
    bc = jnp.cumsum(to_chunks(log_a, GLA_DK_HEAD), axis=3)

    def step(state, inp):
        q_, k_, v_, b_ = inp
        b_tot = b_[:, :, -1:, :]
        k_dec = k_ * jnp.exp(b_tot - b_)
        state = jnp.exp(b_tot[:, :, 0, :, None]) * state + jnp.einsum('bhck,bhcv->bhkv', k_dec, v_)
        o_ = jnp.einsum('bhck,bhkv->bhcv', q_, state)
        return state, o_

    s0 = jnp.zeros((B_, GLA_HEADS, GLA_DK_HEAD, GLA_DV_HEAD), jnp.float32)
    _, o = lax.scan(step, s0, (qc, kc, vc, bc))
    o = o.transpose(1, 0, 3, 2, 4).reshape(B_, S_, GLA_HEADS, GLA_DV_HEAD)
    o = o * lax.rsqrt(jnp.mean(o * o, axis=-1, keepdims=True) + EPS) * norm_g.astype(jnp.float32)
    o = o * jax.nn.silu(g.astype(jnp.float32)).reshape(B_, S_, GLA_HEADS, GLA_DV_HEAD)
    return o.reshape(B_, S_, GLA_DV).astype(h.dtype) @ w_out


def _moe(h, router, w13_all, w2_all, j):
    T, D = h.shape
    logits = (h @ router).astype(jnp.float32)
    top_v, top_i = lax.top_k(logits, TOP_K)
    gates = jax.nn.softmax(top_v, axis=-1)
    TK = T * TOP_K
    flat_e = top_i.reshape(TK)
    flat_tok = jnp.arange(TK, dtype=jnp.int32) // TOP_K
    flat_gate = gates.reshape(TK)
    order = jnp.argsort(flat_e, stable=True)
    sorted_e = flat_e[order]
    counts = jnp.bincount(flat_e, length=N_EXPERTS)
    padded = (counts + MOE_BLOCK - 1) // MOE_BLOCK * MOE_BLOCK
    pad_end = jnp.cumsum(padded)
    pad_start = pad_end - padded
    start = jnp.cumsum(counts) - counts
    dest = pad_start[sorted_e] + jnp.arange(TK, dtype=jnp.int32) - start[sorted_e]
    n_blocks = -(-TK // MOE_BLOCK) + N_EXPERTS
    n_slots = n_blocks * MOE_BLOCK
    slot_tok = jnp.full((n_slots,), T, jnp.int32).at[dest].set(flat_tok[order])
    slot_gate = jnp.zeros((n_slots,), jnp.float32).at[dest].set(flat_gate[order])
    block_start = jnp.arange(n_blocks, dtype=jnp.int32) * MOE_BLOCK
    block_e = jnp.minimum(jnp.sum(block_start[:, None] >= pad_end[None, :], axis=1), N_EXPERTS - 1)
    h_pad = jnp.concatenate([h, jnp.zeros((1, D), h.dtype)], axis=0)

    def expert_block(args):
        tok, e = args
        return _swiglu(h_pad[tok], w13_all[j, e], w2_all[j, e])

    y_slots = lax.map(expert_block, (slot_tok.reshape(n_blocks, MOE_BLOCK), block_e))
    y_slots = y_slots.reshape(n_slots, D) * slot_gate[:, None].astype(h.dtype)
    return jax.ops.segment_sum(y_slots, slot_tok, num_segments=T + 1)[:T]


def setup_inputs(seed: int = 0) -> dict:
    key = jax.random.key(seed)
    ks = jax.random.split(key, 20)
    D = D_MODEL
    f32 = jnp.float32

    def nrm(k, shape, scale):
        return jax.random.normal(k, shape, f32) * scale

    return {
        "x": nrm(ks[0], (BATCH, SEQ, D), 1.0),
        "c": nrm(ks[1], (BATCH, D), 1.0),
        "ada_w": nrm(ks[2], (DEPTH, D, 6 * D), 0.5 * D ** -0.5),
        "ada_b": nrm(ks[3], (DEPTH, 6 * D), 0.01),
        "norm_g": 1.0 + nrm(ks[4], (DEPTH, 2, D), 0.02),
        "conv_w_in": nrm(ks[5], (N_CONV_LAYERS, D, 3 * D), D ** -0.5),
        "conv_k": nrm(ks[6], (N_CONV_LAYERS, CONV_W, D), CONV_W ** -0.5),
        "conv_w_out": nrm(ks[7], (N_CONV_LAYERS, D, D), D ** -0.5),
        "gla_w_in": nrm(ks[8], (N_GLA_LAYERS, D, GLA_IN), D ** -0.5),
        "gla_w_gk": nrm(ks[9], (N_GLA_LAYERS, GLA_GATE_RANK, GLA_DK), GLA_GATE_RANK ** -0.5),
        "gla_b_gk": 2.0 + nrm(ks[10], (N_GLA_LAYERS, GLA_DK), 0.1),
        "gla_norm_g": 1.0 + nrm(ks[11], (N_GLA_LAYERS, GLA_DV_HEAD), 0.02),
        "gla_w_out": nrm(ks[12], (N_GLA_LAYERS, GLA_DV, D), GLA_DV ** -0.5),
        "ffn_w13": nrm(ks[13], (N_CONV_LAYERS, D, 2 * D_FF), D ** -0.5),
        "ffn_w2": nrm(ks[14], (N_CONV_LAYERS, D_FF, D), D_FF ** -0.5),
        "moe_router": nrm(ks[15], (N_GLA_LAYERS, D, N_EXPERTS), D ** -0.5),
        "moe_w13": nrm(ks[16], (N_GLA_LAYERS, N_EXPERTS, D, 2 * D_FF_EXPERT), D ** -0.5),
        "moe_w2": nrm(ks[17], (N_GLA_LAYERS, N_EXPERTS, D_FF_EXPERT, D), D_FF_EXPERT ** -0.5),
        "final_g": 1.0 + nrm(ks[18], (D,), 0.02),
    }


def reference(x, c, ada_w, ada_b, norm_g, conv_w_in, conv_k, conv_w_out,
              gla_w_in, gla_w_gk, gla_b_gk, gla_norm_g, gla_w_out,
              ffn_w13, ffn_w2, moe_router, moe_w13, moe_w2, final_g):
    B_, S_, D = x.shape
    c_act = jax.nn.silu(c)
    for i in range(DEPTH):
        j = i // 2
        mod = (c_act @ ada_w[i] + ada_b[i])[:, None, :]
        sh1, sc1, g1, sh2, sc2, g2 = jnp.split(mod, 6, axis=-1)
        h = _modulate(_rmsnorm(x, norm_g[i, 0]), sh1, sc1)
        if i % 2 == 0:
            m = _conv_mixer(h, conv_w_in[j], conv_k[j], conv_w_out[j])
        else:
            m = _gla_mixer(h, gla_w_in[j], gla_w_gk[j], gla_b_gk[j], gla_norm_g[j], gla_w_out[j])
        x = x + g1 * m
        h = _modulate(_rmsnorm(x, norm_g[i, 1]), sh2, sc2)
        if i % 2 == 0:
            f = _swiglu(h, ffn_w13[j], ffn_w2[j])
        else:
            f = _moe(h.reshape(B_ * S_, D), moe_router[j], moe_w13, moe_w2, j).reshape(B_, S_, D)
        x = x + g2 * f
    return _rmsnorm(x, final_g)
```

```python
import contextlib
import numpy as np
import concourse.bass as bass
import concourse.mybir as mybir
from concourse.bass_utils import run_bass_kernel_spmd

F32 = mybir.dt.float32
BF16 = mybir.dt.bfloat16
AF = mybir.ActivationFunctionType
ALU = mybir.AluOpType
AX = mybir.AxisListType

D = 2048
T = 1024
KC = 16
DFF = 5632
NE = 8
EPS = 1e-6
SEM_ROT = 20000
PAIRS = [[0, 1], [2, 3], [4, 5], [6, 7]]


class Prog:
    def __init__(self, nc, es):
        self.nc = nc
        self.es = es
        self.ins = []
        self.last_w = {}
        self.readers = {}
        self.dsems = {}

    def add(self, eng, fn, reads=(), writes=(), dsem=None, inc=16, mark=None, nofence=False):
        k = len(self.ins)
        deps = set()
        if not nofence and "EPOCH" not in writes:
            reads = tuple(reads) + ("EPOCH",)
        for r in reads:
            w = self.last_w.get(r)
            if w is not None:
                deps.add(w)
        for w_ in writes:
            w = self.last_w.get(w_)
            if w is not None:
                deps.add(w)
            deps |= self.readers.get(w_, set())
        if mark is None:
            for r in reads:
                self.readers.setdefault(r, set()).add(k)
        for w_ in writes:
            self.last_w[w_] = k
            self.readers[w_] = set()
        deps.discard(k)
        self.ins.append(dict(eng=eng, fn=fn, deps=deps, dsem=dsem, inc=inc, mark=mark))
        return k

    def region_begin(self, flag_ap, flagkey):
        for eng in ("pe", "act", "dve", "pool", "sp"):
            self.add(eng, None, reads=(flagkey,), mark=("rb", flag_ap))

    def region_end(self):
        for eng in ("pe", "act", "dve", "pool", "sp"):
            self.add(eng, None, mark=("re",))

    def emit(self):
        nc = self.nc
        ins = self.ins
        n = len(ins)
        needs = [False] * n
        for k, it in enumerate(ins):
            for d in it["deps"]:
                if ins[d]["dsem"] is None and not (ins[d]["eng"] == "pe" and it["eng"] == "pe"):
                    needs[d] = True
        sig = [None] * n
        cnt = {}
        semobjs = {}

        def getsem(name):
            if name not in semobjs:
                semobjs[name] = self.es.enter_context(nc.semaphore(name))
            return semobjs[name]

        dcnt = {}
        for k, it in enumerate(ins):
            if it["dsem"] is not None:
                dcnt[it["dsem"]] = dcnt.get(it["dsem"], 0) + it["inc"]
                sig[k] = (getsem("d_" + it["dsem"]), dcnt[it["dsem"]])
            elif needs[k]:
                e = it["eng"]
                c = cnt.get(e, 0)
                cnt[e] = c + 1
                sig[k] = (getsem(f"c_{e}_{c // SEM_ROT}"), c % SEM_ROT + 1)
        streams = {}
        for k, it in enumerate(ins):
            streams.setdefault(it["eng"], []).append(k)
        self.stats = {e: len(v) for e, v in streams.items()}
        block = self.es.enter_context(nc.Block())

        def run_stream(eng_name, eobj):
            waited = {}
            st_ = streams.get(eng_name, [])
            reg = None
            for pos, k in enumerate(st_):
                it = ins[k]
                if it["mark"] is not None and it["mark"][0] == "re":
                    if reg is None:
                        continue
                    reg["guard"].__exit__(None, None, None)
                    comp = {}
                    for kk in reg["members"]:
                        if sig[kk] is not None:
                            s_, v_ = sig[kk]
                            inc_ = ins[kk]["inc"] if ins[kk]["dsem"] is not None else 1
                            c_ = comp.setdefault(id(s_), [s_, v_ - inc_, 0, ins[kk]["dsem"] is not None])
                            c_[2] += inc_
                    if comp:
                        with eobj.Else():
                            if any(not c_[3] for c_ in comp.values()):
                                eobj.drain()
                            for s_, before, tot, isd in comp.values():
                                if isd and before > 0:
                                    eobj.wait_ge(s_, before)
                                eobj.sem_inc(s_, tot)
                    waited = reg["waited"]
                    reg["rctx"].__exit__(None, None, None)
                    reg = None
                    continue
                if it["mark"] is not None and it["mark"][0] == "rb":
                    members = []
                    for k2 in st_[pos + 1:]:
                        if ins[k2]["mark"] is not None:
                            break
                        members.append(k2)
                    if not members:
                        continue
                want = {}
                for d in it["deps"]:
                    if ins[d]["dsem"] is None and ins[d]["eng"] == "pe" and eng_name == "pe":
                        continue
                    s, v = sig[d]
                    key = id(s)
                    if key not in want or want[key][1] < v:
                        want[key] = (s, v)
                for key, (s, v) in want.items():
                    if waited.get(key, 0) < v:
                        eobj.wait_ge(s, v)
                        waited[key] = v
                if it["mark"] is not None:
                    rctx = eobj.register(f"rf_{eng_name}_{k}")
                    r_ = rctx.__enter__()
                    eobj.reg_load(r_, it["mark"][1])
                    guard = eobj.If_ne(r_, 0)
                    guard.__enter__()
                    reg = dict(guard=guard, members=members, waited=dict(waited), rctx=rctx)
                    continue
                bi = it["fn"](eobj)
                if sig[k] is not None:
                    if it["dsem"] is not None:
                        bi.then_inc(sig[k][0], it["inc"])
                    else:
                        bi.then_inc(sig[k][0], 1)

        @block.tensor
        def _(e):
            run_stream("pe", e)

        @block.scalar
        def _(e):
            run_stream("act", e)

        @block.vector
        def _(e):
            run_stream("dve", e)

        @block.gpsimd
        def _(e):
            run_stream("pool", e)

        @block.sync
        def _(e):
            run_stream("sp", e)


def build_program(plan, final_norm, out_mode="x"):
    nc = bass.Bass("TRN2", target_bir_lowering=False)
    es = contextlib.ExitStack()
    P = Prog(nc, es)
    layers = sorted({i for _, i in plan})

    def din(name, shape, dt=F32):
        return nc.dram_tensor(name, list(shape), dt, kind="ExternalInput").ap()

    x_d = din("xT", [D, T])
    out_d = nc.dram_tensor("outT", [D, T], F32, kind="ExternalOutput").ap()
    cT_d = din("cT", [128, KC])
    adab_d = din("ada_bT", [4, 128, 96])
    ng_d = din("norm_gT", [128, 128])
    fg_d = din("final_gT", [128, KC])
    ck_d = din("conv_kT", [128, 96])
    ident_d = din("ident", [128, 128])
    ones_d = din("ones", [128, 128])
    u2_d = din("u2", [128, 128])
    ind2_d = din("ind2", [128, 2])
    flag_d = din("flag", [128, 1])
    lt_d = din("lt", [128, 128])
    iota_d = din("iota", [128, 512])
    W = {}
    for i in layers:
        j = i // 2
        W[("ada", i)] = din(f"ada_w{i}", [D, 6 * D])
    for kind, i in plan:
        j = i // 2
        if kind == "mixer" and i % 2 == 0:
            W[("cin", i)] = din(f"conv_w_in{j}", [D, 3 * D])
            W[("cout", i)] = din(f"conv_w_out{j}", [D, D])
        if kind == "mixer" and i % 2 == 1:
            W[("gin", i)] = din(f"gla_w_in{j}", [D, 6160])
            W[("gout", i)] = din(f"gla_w_out{j}", [D, D])
            W[("wgk", i)] = din(f"gla_wgk{j}", [17, 1024])
            W[("gng", i)] = din(f"gla_ng{j}", [128, 512])
        if kind == "ffn" and i % 2 == 0:
            W[("w13", i)] = din(f"ffn_w13_{j}", [D, 2 * DFF])
            W[("w2", i)] = din(f"ffn_w2_{j}", [DFF, D])
        if kind == "ffn" and i % 2 == 1:
            W[("rt", i)] = din(f"moe_router{j}", [128, KC, NE])
            W[("mw13", i)] = din(f"moe_w13_{j}", [NE, D, 2 * DFF])
            W[("mw2", i)] = din(f"moe_w2_{j}", [NE, DFF, D])
    cc_h_in = nc.dram_tensor("cc_h_in", [128, 32], BF16)
    cc_h_out = nc.dram_tensor("cc_h_out", [256, 32], BF16)
    cc_s_in = nc.dram_tensor("cc_s_in", [128, 1024], F32)
    cc_s_out = nc.dram_tensor("cc_s_out", [256, 1024], F32)

    def sb(name, shape, dt):
        return es.enter_context(nc.sbuf_tensor(name, list(shape), dt))

    xT = sb("xT_s", [128, KC, T], F32)
    hT = sb("hT_s", [128, KC, T], BF16)
    ws = [sb(f"ws{i}", [128, 8192], BF16) for i in range(3)]
    ARENA = 24640
    arena = sb("arena", [128, ARENA], BF16)
    ident = sb("ident_s", [128, 128], F32)
    ones = sb("ones_s", [128, 128], F32)
    u2 = sb("u2_s", [128, 128], F32)
    ind2 = sb("ind2_s", [128, 2], F32)
    flag = sb("flag_s", [128, 1], F32)
    lt = sb("lt_s", [128, 128], F32)
    identb = sb("identb", [128, 128], BF16)
    msm = sb("msm", [128, 512], F32)
    wtb = sb("wtb", [128, 64], BF16)
    gslot = sb("gslot", [128, 4], F32)
    flags_all = [sb(f"flags_i{q}", [1, 32], mybir.dt.int32) for q in range(2)]
    cntf = sb("cntf", [1, 8], F32)
    dummy = sb("dummy_f", [1, 8], F32)
    modT = sb("modT", [128, 4 * 96], F32)
    ngT = sb("ngT", [128, 128], F32)
    fgT = sb("fgT", [128, KC], F32)
    ckT = sb("ckT", [128, 96], F32)
    craw = sb("craw", [128, KC], F32)
    cact = sb("cact", [128, KC], BF16)
    AB = sb("AB", [128, 6 * KC], F32)
    nt = [sb(f"nt{i}", [128, 512], F32) for i in range(2)]
    rstd = sb("rstd", [128, 512], F32)
    sa = [sb(f"sa{i}", [128, 512], BF16) for i in range(2)]
    ps = [es.enter_context(nc.psum_tensor(f"ps{i}", [128, 512], F32)) for i in range(8)]

    def PS(b):
        return ("ps", b)

    modrow = [arena[0:1, i * 1024:(i + 1) * 1024].bitcast(F32) for i in range(2)]
    adab = arena[:, 4096:4096 + 768].bitcast(F32)

    def carve(off, n_bf16, dt, pattern=None, **kw):
        v = arena[:, off:off + n_bf16]
        if dt == F32:
            v = v.bitcast(F32)
        if pattern:
            v = v.rearrange(pattern, **kw)
        return v

    tasks = []

    def task(dmas, fn):
        tasks.append((dmas, fn))

    def wview(s, nk, w):
        return ws[s][:, 0:nk * w].rearrange("p (k w) -> p k w", w=w)

    def run_tasks():
        widx = [i for i, t_ in enumerate(tasks) if t_[0] not in ("rb", "re") and t_[0] is not None]
        slot_of = {ti: n % 2 for n, ti in enumerate(widx)}
        wpos = {ti: n for n, ti in enumerate(widx)}
        issued = [0]

        def issue_upto(nw, limit_ti):
            while issued[0] < min(nw, len(widx)) and widx[issued[0]] < limit_ti:
                ti = widx[issued[0]]
                s = slot_of[ti]
                dl = tasks[ti][0]
                for qi, (nk, w, off, ncols, src) in enumerate(dl):
                    dst = wview(s, nk, w)[:, :, off:off + ncols]
                    wk = (("w", s, qi),) if len(dl) == 2 else (("w", s, 0), ("w", s, 1))
                    P.add("pool", (lambda e, dst=dst, src=src: e.dma_start(out=dst, in_=src)),
                          reads=(), writes=wk, dsem=f"w{s}", nofence=True)
                issued[0] += 1

        marks = [i for i, t_ in enumerate(tasks) if t_[0] in ("rb", "re")] + [len(tasks)]
        nw_done = 0
        for ti, t_ in enumerate(tasks):
            if t_[0] == "rb":
                P.region_begin(t_[1], t_[2])
                continue
            if t_[0] == "re":
                P.region_end()
                continue
            limit = min(m for m in marks if m > ti)
            dmas, fn = t_
            if dmas is not None:
                issue_upto(nw_done + 2, limit)
                fn(slot_of[ti])
                nw_done += 1
            else:
                issue_upto(nw_done + 1, limit)
                fn(None)

    def wsrc(w_ap, r0, nk, c0, ncols):
        return w_ap[r0:r0 + nk * 128, c0:c0 + ncols].rearrange("(k p) n -> p k n", p=128)

    def mm(out, lhsT, rhs, start, stop, reads, writes):
        P.add("pe", (lambda e: e.matmul(out, lhsT, rhs, start=start, stop=stop)), reads=reads, writes=writes)

    def act(out, in_, func, reads, writes, **kw):
        P.add("act", (lambda e: e.activation(out=out, in_=in_, func=func, **kw)), reads=reads, writes=writes)

    def dve(fn, reads, writes):
        P.add("dve", fn, reads=reads, writes=writes)

    def sp_dma(out, in_, reads, writes, dsem):
        P.add("sp", (lambda e: e.dma_start(out=out, in_=in_)), reads=reads, writes=writes, dsem=dsem)

    def load_consts(_):
        for dst, src, key in [(ident, ident_d, "ident"), (ones, ones_d, "ones"), (u2, u2_d, "u2"),
                              (ind2, ind2_d, "ind2"), (flag, flag_d, "flag"), (ngT, ng_d, "ngT"),
                              (fgT, fg_d, "fgT"), (ckT, ck_d, "ckT"), (craw, cT_d, "craw"), (lt, lt_d, "lt")]:
            sp_dma(dst[:], src, (), (key,), "c_" + key)
        for i in layers:
            sp_dma(adab[:, i * 96:(i + 1) * 96], adab_d[i], (), (("adab", i),), f"c_adab{i}")
        for kc in range(KC):
            sp_dma(xT[:, kc, :], x_d[kc * 128:(kc + 1) * 128, :], (), tuple(("x", kc, tt) for tt in range(2)), f"x{kc}")
        act(cact[:], craw[:], AF.Silu, ("craw",), ("cact",))
        P.add("pool", (lambda e: e.dma_start(out=identb[:], in_=ident_d)), reads=(), writes=("identb",), dsem="c_identb")

    task(None, load_consts)

    def ada_layer(i):
        wa = W[("ada", i)]
        PSB = 7

        def blk(nb):
            def fn(s):
                wv = wview(s, KC, 512)
                b = nb % 2
                for kc in range(KC):
                    mm(ps[b][0:1, :], cact[:, kc:kc + 1], wv[:, kc, :], kc == 0, kc == KC - 1,
                       (("w", s, 0), ("w", s, 1), "cact"), (PS(b),))
                mr = modrow[nb % 2]
                act(mr, ps[b][0:1, :], AF.Copy, (PS(b),), (("modrow", nb % 2),))
                for q in range(4):
                    col = nb * 4 + q
                    mm(ps[PSB][:, col:col + 1], mr[0:1, q * 128:(q + 1) * 128], ones[0:1, 0:1], True, True,
                       (("modrow", nb % 2), "ones"), (PS(PSB),))
                if nb == 23:
                    dve(lambda e: e.tensor_tensor(out=modT[:, i * 96:(i + 1) * 96], in0=ps[PSB][:, 0:96],
                                                  in1=adab[:, i * 96:(i + 1) * 96], op=ALU.add),
                        (PS(PSB), ("adab", i)), (("mod", i),))
            return fn

        for nb in range(24):
            task([(KC, 512, 0, 512, wsrc(wa, 0, KC, nb * 512, 512))], blk(nb))

    def m_col(i, sec):
        return modT[:, i * 96 + sec * 16: i * 96 + (sec + 1) * 16]

    def prep_AB(i):
        def fn(_):
            for s_ in range(2):
                ng = ngT[:, i * 32 + s_ * 16: i * 32 + (s_ + 1) * 16]
                sc = m_col(i, 3 * s_ + 1)
                A = AB[:, (3 * s_) * 16:(3 * s_ + 1) * 16]
                dve(lambda e, A=A, sc=sc, ng=ng: e.scalar_tensor_tensor(out=A, in0=sc, scalar=1.0, in1=ng,
                                                                         op0=ALU.add, op1=ALU.mult),
                    (("mod", i), "ngT"), (("AB", 3 * s_),))
                B = AB[:, (3 * s_ + 1) * 16:(3 * s_ + 2) * 16]
                dve(lambda e, B=B, sh=m_col(i, 3 * s_): e.tensor_copy(out=B, in_=sh),
                    (("mod", i),), (("AB", 3 * s_ + 1),))
                G = AB[:, (3 * s_ + 2) * 16:(3 * s_ + 3) * 16]
                dve(lambda e, G=G, g=m_col(i, 3 * s_ + 2): e.tensor_copy(out=G, in_=g),
                    (("mod", i),), (("AB", 3 * s_ + 2),))
        task(None, fn)

    def ABc(idx, kc):
        return AB[:, idx * 16 + kc: idx * 16 + kc + 1]

    def norm_stats(tt, bank):
        for kc in range(KC):
            b = kc % 2
            act(nt[b][:], xT[:, kc, tt * 512:(tt + 1) * 512], AF.Square, (("x", kc, tt),), (("nt", b),))
            mm(ps[bank][:], ones[:], nt[b][:], kc == 0, kc == KC - 1, (("nt", b), "ones"), (PS(bank),))
        act(rstd[:], ps[bank][:], AF.Sqrt, (PS(bank),), ("rstd",), scale=1.0 / D, bias=EPS)
        dve(lambda e: e.reciprocal(out=rstd[:], in_=rstd[:]), ("rstd",), ("rstd",))

    def norm_mod(s_, extra=None):
        def fn(_):
            for tt in range(2):
                norm_stats(tt, 6)
                for kc in range(KC):
                    b = kc % 2
                    dve(lambda e, b=b, kc=kc, tt=tt: e.scalar_tensor_tensor(
                        out=nt[b][:], in0=xT[:, kc, tt * 512:(tt + 1) * 512], scalar=ABc(3 * s_, kc),
                        in1=rstd[:], op0=ALU.mult, op1=ALU.mult),
                        (("x", kc, tt), "rstd", ("AB", 3 * s_)), (("nt", b),))
                    act(hT[:, kc, tt * 512:(tt + 1) * 512], nt[b][:], AF.Identity,
                        (("nt", b), ("AB", 3 * s_ + 1)), (("h", kc, tt),), bias=ABc(3 * s_ + 1, kc), scale=1.0)
                if extra is not None:
                    extra(tt)
        task(None, fn)

    SECTIONS = [(0, 12), (12, 12), (24, 10), (34, 10)]
    ACT_OFF = 0
    actb = carve(ACT_OFF, 12 * 1024, BF16, "p (c t) -> p c t", t=T)
    WB_OFF = 12288
    Wb = carve(WB_OFF, 8 * 1024, BF16, "p (e t) -> p e t", t=T)
    sa2 = [carve(20480 + i * 512, 512, BF16) for i in range(2)]
    up_rot = [0]
    dn_rot = [0]

    def ffn(w13, w2, gidx, gate_e=None):
        for (c0, n) in SECTIONS:
            for blk in range(n // 2):
                c = c0 + 2 * blk

                def fn(s, c=c, c0=c0):
                    wv = wview(s, KC, 512)
                    for ci in range(2):
                        for tt in range(2):
                            r = up_rot[0] % 3
                            up_rot[0] += 1
                            ba, bb = 2 * r, 2 * r + 1
                            for kc in range(KC):
                                mm(ps[ba][:], wv[:, kc, ci * 128:(ci + 1) * 128], hT[:, kc, tt * 512:(tt + 1) * 512],
                                   kc == 0, kc == KC - 1, (("w", s, 0), ("w", s, 1), ("h", kc, tt)), (PS(ba),))
                                mm(ps[bb][:], wv[:, kc, 256 + ci * 128:256 + (ci + 1) * 128],
                                   hT[:, kc, tt * 512:(tt + 1) * 512],
                                   kc == 0, kc == KC - 1, (("w", s, 0), ("w", s, 1), ("h", kc, tt)), (PS(bb),))
                            sb_ = r % 2
                            act(sa[sb_][:], ps[ba][:], AF.Silu, (PS(ba),), (("sa", sb_),))
                            dst = actb[:, c + ci - c0, tt * 512:(tt + 1) * 512]
                            if gate_e is None:
                                dve(lambda e, dst=dst, sb_=sb_, bb=bb: e.tensor_tensor(out=dst, in0=ps[bb][:], in1=sa[sb_][:], op=ALU.mult),
                                    (PS(bb), ("sa", sb_)), (("act", c + ci - c0, tt),))
                            else:
                                dve(lambda e, sb_=sb_, tt=tt: e.tensor_tensor(out=sa2[sb_], in0=sa[sb_][:], in1=Wb[:, gate_e, tt * 512:(tt + 1) * 512], op=ALU.mult),
                                    (("sa", sb_), ("Wb", gate_e, tt)), (("sa2", sb_),))
                                dve(lambda e, dst=dst, sb_=sb_, bb=bb: e.tensor_tensor(out=dst, in0=ps[bb][:], in1=sa2[sb_], op=ALU.mult),
                                    (PS(bb), ("sa2", sb_)), (("act", c + ci - c0, tt),))
                task([(KC, 512, 0, 256, wsrc(w13, 0, KC, c * 128, 256)),
                      (KC, 512, 256, 256, wsrc(w13, 0, KC, DFF + c * 128, 256))], fn)
            for nb in range(4):
                def fn2(s, nb=nb, n=n):
                    wv = wview(s, n, 512)
                    for nci in range(4):
                        for tt in range(2):
                            b = 6 + dn_rot[0] % 2
                            dn_rot[0] += 1
                            for k in range(n):
                                mm(ps[b][:], wv[:, k, nci * 128:(nci + 1) * 128], actb[:, k, tt * 512:(tt + 1) * 512],
                                   k == 0, k == n - 1, (("w", s, 0), ("w", s, 1), ("act", k, tt)), (PS(b),))
                            kc = nb * 4 + nci
                            xs = xT[:, kc, tt * 512:(tt + 1) * 512]
                            dve(lambda e, xs=xs, b=b, kc=kc: e.scalar_tensor_tensor(out=xs, in0=ps[b][:], scalar=ABc(gidx, kc), in1=xs,
                                                                                    op0=ALU.mult, op1=ALU.add),
                                (PS(b), ("x", kc, tt), ("AB", gidx)), (("x", kc, tt),))
                task([(n, 512, 0, 512, wsrc(w2, c0 * 128, n, nb * 512, 512))], fn2)

    def conv_mixer(i):
        j = i // 2
        win, wout = W[("cin", i)], W[("cout", i)]
        VW = T + 2
        vT = carve(0, KC * VW, BF16, "p (c t) -> p c t", t=VW)
        cg = [carve(16416 + q * 4096, 4096, BF16, "p (c t) -> p c t", t=T) for q in range(2)]
        halo = carve(24608, 32, BF16, "p (c t) -> p c t", t=2)
        ctmp = [nt[0], nt[1]]
        rot = [0]
        for blk in range(8):
            c = 2 * blk

            def fn(s, c=c):
                wv = wview(s, KC, 512)
                for ci in range(2):
                    for tt in range(2):
                        r = rot[0] % 3
                        rot[0] += 1
                        ba, bb = 2 * r, 2 * r + 1
                        for kc in range(KC):
                            mm(ps[ba][:], wv[:, kc, ci * 128:(ci + 1) * 128], hT[:, kc, tt * 512:(tt + 1) * 512],
                               kc == 0, kc == KC - 1, (("w", s, 0), ("w", s, 1), ("h", kc, tt)), (PS(ba),))
                            mm(ps[bb][:], wv[:, kc, 256 + ci * 128:256 + (ci + 1) * 128], hT[:, kc, tt * 512:(tt + 1) * 512],
                               kc == 0, kc == KC - 1, (("w", s, 0), ("w", s, 1), ("h", kc, tt)), (PS(bb),))
                        sb_ = r % 2
                        act(sa[sb_][:], ps[ba][:], AF.Copy, (PS(ba),), (("sa", sb_),))
                        dst = vT[:, c + ci, 2 + tt * 512:2 + (tt + 1) * 512]
                        dve(lambda e, dst=dst, sb_=sb_, bb=bb: e.tensor_tensor(out=dst, in0=ps[bb][:], in1=sa[sb_][:], op=ALU.mult),
                            (PS(bb), ("sa", sb_)), (("v", c + ci, tt),))
            task([(KC, 512, 0, 256, wsrc(win, 0, KC, D + c * 128, 256)),
                  (KC, 512, 256, 256, wsrc(win, 0, KC, 2 * D + c * 128, 256))], fn)

        def exch(_):
            sp_dma(cc_h_in.ap().rearrange("p (c t) -> p c t", t=2), vT[:, :, T:T + 2],
                   tuple(("v", c, 1) for c in range(KC)), ("cc_h_in",), f"hx{i}a")
            P.add("pool", (lambda e: e.collective_compute("AllGather", ALU.bypass, replica_groups=PAIRS,
                                                          ins=[cc_h_in.ap().opt()], outs=[cc_h_out.ap().opt()])),
                  reads=("cc_h_in",), writes=("cc_h_out",), dsem=f"hx{i}b", inc=1)
            sp_dma(halo, cc_h_out.ap()[0:128, :].rearrange("p (c t) -> p c t", t=2), ("cc_h_out",), ("halo",), f"hx{i}c")
            dve(lambda e: e.tensor_scalar(out=vT[:, :, 0:2], in0=halo, scalar1=flag[:, 0:1], scalar2=None, op0=ALU.mult),
                ("halo", "flag"), tuple(("vh", c) for c in range(KC)))
        task(None, exch)

        def ck(tap, kc):
            o = j * 48 + tap * 16 + kc
            return ckT[:, o:o + 1]

        for c4 in range(4):
            def fn(s, c4=c4):
                wv = wview(s, KC, 512)
                for ci in range(4):
                    c = c4 * 4 + ci
                    for tt in range(2):
                        r = rot[0] % 3
                        rot[0] += 1
                        ba = 2 * r
                        for kc in range(KC):
                            mm(ps[ba][:], wv[:, kc, ci * 128:(ci + 1) * 128], hT[:, kc, tt * 512:(tt + 1) * 512],
                               kc == 0, kc == KC - 1, (("w", s, 0), ("w", s, 1), ("h", kc, tt)), (PS(ba),))
                        tb = (2 * ci + tt) % 2
                        tmp = ctmp[tb]
                        rd = (("v", c, tt), ("v", c, 0), ("vh", c), "ckT")
                        o = tt * 512
                        dve(lambda e, tmp=tmp, c=c, o=o: e.tensor_scalar(out=tmp[:], in0=vT[:, c, o + 2:o + 514], scalar1=ck(2, c), scalar2=None, op0=ALU.mult),
                            rd, (("nt", tb),))
                        dve(lambda e, tmp=tmp, c=c, o=o: e.scalar_tensor_tensor(out=tmp[:], in0=vT[:, c, o + 1:o + 513], scalar=ck(1, c), in1=tmp[:], op0=ALU.mult, op1=ALU.add),
                            rd + (("nt", tb),), (("nt", tb),))
                        dve(lambda e, tmp=tmp, c=c, o=o: e.scalar_tensor_tensor(out=tmp[:], in0=vT[:, c, o:o + 512], scalar=ck(0, c), in1=tmp[:], op0=ALU.mult, op1=ALU.add),
                            rd + (("nt", tb),), (("nt", tb),))
                        dst = cg[c4 % 2][:, ci, o:o + 512]
                        dve(lambda e, dst=dst, tmp=tmp, ba=ba: e.tensor_tensor(out=dst, in0=ps[ba][:], in1=tmp[:], op=ALU.mult),
                            (PS(ba), ("nt", tb)), (("cg", c4 % 2, ci, tt),))
            task([(KC, 512, 0, 512, wsrc(win, 0, KC, c4 * 512, 512))], fn)

            def fn2(s, c4=c4):
                wv = wview(s, 4, 2048)
                for nci in range(KC):
                    for tt in range(2):
                        b = 6 + dn_rot[0] % 2
                        dn_rot[0] += 1
                        for k in range(4):
                            mm(ps[b][:], wv[:, k, nci * 128:(nci + 1) * 128], cg[c4 % 2][:, k, tt * 512:(tt + 1) * 512],
                               k == 0, k == 3, (("w", s, 0), ("w", s, 1), ("cg", c4 % 2, k, tt)), (PS(b),))
                        xs = xT[:, nci, tt * 512:(tt + 1) * 512]
                        dve(lambda e, xs=xs, b=b, nci=nci: e.scalar_tensor_tensor(out=xs, in0=ps[b][:], scalar=ABc(2, nci), in1=xs, op0=ALU.mult, op1=ALU.add),
                            (PS(b), ("x", nci, tt), ("AB", 2)), (("x", nci, tt),))
            task([(4, 2048, 0, 2048, wsrc(wout, c4 * 512, 4, 0, 2048))], fn2)


    def gla_mixer(i):
        j = i // 2
        win, wout, wgk_d, gng_d = W[("gin", i)], W[("gout", i)], W[("wgk", i)], W[("gng", i)]
        qT = carve(0, 2048, BF16, "p (k t) -> p k t", t=T)
        kdec = carve(2048, 2048, BF16, "p (a n) -> p a n", n=256)
        vbf = carve(4096, 4096, BF16, "p (a n) -> p a n", n=512)
        sg = carve(8192, 4096, BF16, "p (a n) -> p a n", n=512)
        S = carve(12288, 2048, F32, "p (k n) -> p k n", n=512)
        S2 = carve(12288, 2048, F32)
        Sbf = carve(14336, 1024, BF16, "p (k n) -> p k n", n=512)
        oT = carve(15360, 4096, BF16, "p (k t) -> p k t", t=T)
        og = carve(19456, 1024, F32)
        spb = [carve(20480 + q * 512, 512, F32) for q in range(2)]
        etb = [carve(21504 + q * 512, 512, F32) for q in range(2)]
        gkT = arena[0:32, 22528:24576].bitcast(F32)
        dec = carve(24576, 64, F32)
        ngb, wgk = nt[0], nt[1]

        def setup(_):
            dve(lambda e: e.memset(gkT, 1.0), (), ("gkT",))
            sp_dma(ngb[:], gng_d, (), (("nt", 0),), f"gng{i}")
        task(None, setup)

        def t0(s):
            wv = wview(s, KC, 16)
            for tt in range(2):
                for kc in range(KC):
                    mm(ps[7][0:16, :], wv[:, kc, 0:16], hT[:, kc, tt * 512:(tt + 1) * 512], kc == 0, kc == KC - 1,
                       (("w", s, 0), ("w", s, 1), ("h", kc, tt)), (PS(7),))
                act(gkT[0:16, tt * 512:(tt + 1) * 512], ps[7][0:16, :], AF.Copy, (PS(7),), ("gkT",))
        task([(KC, 16, 0, 16, wsrc(win, 0, KC, 6144, 16))], t0)

        for h in range(4):
            def tq(s, h=h):
                wv = wview(s, KC, 256)
                for dkc in range(2):
                    for tt in range(2):
                        b = 4 + (2 * dkc + tt) % 4
                        for kc in range(KC):
                            mm(ps[b][:], wv[:, kc, dkc * 128:(dkc + 1) * 128], hT[:, kc, tt * 512:(tt + 1) * 512], kc == 0, kc == KC - 1,
                               (("w", s, 0), ("w", s, 1), ("h", kc, tt)), (PS(b),))
                        act(qT[:, dkc, tt * 512:(tt + 1) * 512], ps[b][:], AF.Copy, (PS(b),), (("q", dkc, tt),), scale=1.0 / 16.0)
            task([(KC, 256, 0, 256, wsrc(win, 0, KC, h * 256, 256))], tq)

            def tk(s, h=h):
                wv = wview(s, KC, 256)
                sp_dma(wgk[0:17, 0:256], wgk_d[:, h * 256:(h + 1) * 256], (), (("nt", 1),), f"wgk{i}")
                for tile in range(8):
                    bk, bz, bD = tile % 2, 2 + tile % 2, 4 + tile % 2
                    tk_ = slice(tile * 128, (tile + 1) * 128)
                    for kc in range(KC):
                        mm(ps[bk][:, 0:256], hT[:, kc, tk_], wv[:, kc, 0:256], kc == 0, kc == KC - 1,
                           (("w", s, 0), ("w", s, 1), ("h", kc, tile // 4)), (PS(bk),))
                    mm(ps[bz][:, 0:256], gkT[0:17, tk_], wgk[0:17, 0:256], True, True, ("gkT", ("nt", 1)), (PS(bz),))
                    et, sp_ = etb[tile % 2], spb[tile % 2]
                    act(et, ps[bz][:, 0:256], AF.Exp, (PS(bz),), (("et", tile % 2),), scale=-1.0)
                    act(sp_, et, AF.Ln, (("et", tile % 2),), (("sp", tile % 2),), bias=1.0)
                    mm(ps[bD][:, 0:256], u2[:], sp_, True, True, ("u2", ("sp", tile % 2)), (PS(bD),))
                    for dkc in range(2):
                        col = dkc * 16 + tile * 2
                        mm(ps[6][:, col:col + 2], sp_[:, dkc * 128:(dkc + 1) * 128], ind2[:], True, True,
                           ("ind2", ("sp", tile % 2)), (PS(6),))
                    act(et, ps[bD][:, 0:256], AF.Exp, (PS(bD),), (("et", tile % 2),))
                    dve(lambda e, tile=tile, bk=bk, et=et: e.tensor_tensor(out=kdec[:, tile, :], in0=ps[bk][:, 0:256], in1=et, op=ALU.mult),
                        (PS(bk), ("et", tile % 2)), (("kdec", tile),))
                act(dec, ps[6][:, 0:32], AF.Exp, (PS(6),), ("dec",))
            task([(KC, 256, 0, 256, wsrc(win, 0, KC, 1024 + h * 256, 256))], tk)

            def tv(s, h=h):
                wv = wview(s, KC, 512)
                for tile in range(8):
                    b = tile % 2
                    for kc in range(KC):
                        mm(ps[b][:], hT[:, kc, tile * 128:(tile + 1) * 128], wv[:, kc, :], kc == 0, kc == KC - 1,
                           (("w", s, 0), ("w", s, 1), ("h", kc, tile // 4)), (PS(b),))
                    act(vbf[:, tile, :], ps[b][:], AF.Copy, (PS(b),), (("gv", tile),))
            task([(KC, 512, 0, 512, wsrc(win, 0, KC, 2048 + h * 512, 512))], tv)

            def tg(s, h=h):
                wv = wview(s, KC, 512)
                for tile in range(8):
                    b = 2 + tile % 2
                    for kc in range(KC):
                        mm(ps[b][:], hT[:, kc, tile * 128:(tile + 1) * 128], wv[:, kc, :], kc == 0, kc == KC - 1,
                           (("w", s, 0), ("w", s, 1), ("h", kc, tile // 4)), (PS(b),))
                    act(sg[:, tile, :], ps[b][:], AF.Silu, (PS(b),), (("sg", tile),))
            task([(KC, 512, 0, 512, wsrc(win, 0, KC, 4096 + h * 512, 512))], tg)

            def kv_step(c):
                tile, half = c // 2, c % 2
                rows = slice(half * 64, half * 64 + 64)
                for dkc in range(2):
                    b = (c % 2) * 2 + dkc
                    mm(ps[b][:], kdec[rows, tile, dkc * 128:(dkc + 1) * 128], vbf[rows, tile, :], True, True,
                       (("kdec", tile), ("gv", tile)), (PS(b),))
                    dve(lambda e, dkc=dkc, b=b, c=c: e.scalar_tensor_tensor(out=S[:, dkc, :], in0=S[:, dkc, :], scalar=dec[:, dkc * 16 + c:dkc * 16 + c + 1],
                                                                            in1=ps[b][:], op0=ALU.mult, op1=ALU.add),
                        (PS(b), ("S", dkc), "dec"), (("S", dkc),))

            def scan1(_, h=h):
                dve(lambda e: e.memset(S2, 0.0), (), (("S", 0), ("S", 1)))
                for c in range(16):
                    kv_step(c)
                sp_dma(cc_s_in.ap(), S2, (("S", 0), ("S", 1)), ("cc_s_in",), f"sx{i}a")
                P.add("pool", (lambda e: e.collective_compute("AllGather", ALU.bypass, replica_groups=PAIRS,
                                                              ins=[cc_s_in.ap().opt()], outs=[cc_s_out.ap().opt()])),
                      reads=("cc_s_in",), writes=("cc_s_out",), dsem=f"sx{i}b", inc=1)
                sp_dma(S2, cc_s_out.ap()[0:128, :], ("cc_s_out",), (("S", 0), ("S", 1)), f"sx{i}c")
                dve(lambda e: e.tensor_scalar(out=S2, in0=S2, scalar1=flag[:, 0:1], scalar2=None, op0=ALU.mult),
                    (("S", 0), ("S", 1), "flag"), (("S", 0), ("S", 1)))
            task(None, scan1)

            def scan2(_, h=h):
                for c in range(16):
                    tile, half = c // 2, c % 2
                    rows = slice(half * 64, half * 64 + 64)
                    kv_step(c)
                    bo = 4 + c % 2
                    for dkc in range(2):
                        act(Sbf[:, dkc, :], S[:, dkc, :], AF.Copy, (("S", dkc),), (("Sbf", dkc),))
                        mm(ps[bo][:], qT[:, dkc, tile * 128:(tile + 1) * 128], Sbf[:, dkc, :], dkc == 0, dkc == 1,
                           (("q", dkc, tile // 4), ("Sbf", dkc)), (PS(bo),))
                    ssc = rstd[rows, half:half + 1]
                    act(og[rows, :], ps[bo][rows, :], AF.Square, (PS(bo),), (("og", half),))
                    dve(lambda e, rows=rows, ssc=ssc: e.reduce_sum(out=ssc, in_=og[rows, :], axis=AX.X), (("og", half),), (("ss", half),))
                    act(ssc, ssc, AF.Sqrt, (("ss", half),), (("ss", half),), scale=1.0 / 512.0, bias=EPS)
                    dve(lambda e, ssc=ssc: e.reciprocal(out=ssc, in_=ssc), (("ss", half),), (("ss", half),))
                    dve(lambda e, rows=rows, ssc=ssc, bo=bo: e.scalar_tensor_tensor(out=og[rows, :], in0=ps[bo][rows, :], scalar=ssc, in1=ngb[rows, :],
                                                                                     op0=ALU.mult, op1=ALU.mult),
                        (PS(bo), ("ss", half), ("nt", 0)), (("og", half),))
                    dve(lambda e, rows=rows, tile=tile: e.tensor_tensor(out=og[rows, :], in0=og[rows, :], in1=sg[rows, tile, :], op=ALU.mult),
                        (("og", half), ("sg", tile)), (("og", half),))
                    if half == 1:
                        bt = 6 + tile % 2
                        for jx in range(4):
                            P.add("pe", (lambda e, bt=bt, jx=jx: e.transpose(ps[bt][:, jx * 128:(jx + 1) * 128], og[:, jx * 128:(jx + 1) * 128], ident[:])),
                                  reads=(("og", 0), ("og", 1), "ident"), writes=(PS(bt),))
                        act(oT[:, :, tile * 128:(tile + 1) * 128], ps[bt][:].rearrange("p (k t) -> p k t", t=128), AF.Copy,
                            (PS(bt),), (("oT", tile),))
            task(None, scan2)

            def tout(s, h=h):
                wv = wview(s, 4, 2048)
                for nci in range(KC):
                    for tt in range(2):
                        b = 2 + dn_rot[0] % 2
                        dn_rot[0] += 1
                        for k in range(4):
                            mm(ps[b][:], wv[:, k, nci * 128:(nci + 1) * 128], oT[:, k, tt * 512:(tt + 1) * 512], k == 0, k == 3,
                               (("w", s, 0), ("w", s, 1)) + tuple(("oT", tt * 4 + q) for q in range(4)), (PS(b),))
                        xs = xT[:, nci, tt * 512:(tt + 1) * 512]
                        dve(lambda e, xs=xs, b=b, nci=nci: e.scalar_tensor_tensor(out=xs, in0=ps[b][:], scalar=ABc(2, nci), in1=xs, op0=ALU.mult, op1=ALU.add),
                            (PS(b), ("x", nci, tt), ("AB", 2)), (("x", nci, tt),))
            task([(4, 2048, 0, 2048, wsrc(wout, h * 512, 4, 0, 2048))], tout)

    def fence(keys=()):
        dve(lambda e: e.memset(dummy[:], 0.0), (), ("EPOCH",))

    def moe(i):
        j = i // 2
        rt_d, mw13, mw2 = W[("rt", i)], W[("mw13", i)], W[("mw2", i)]
        flags_i = flags_all[j]
        fkey = ("flags", j)
        RB = 16384
        Rr = carve(RB, 256, F32, "p (k e) -> p k e", e=NE)
        RA = carve(RB + 256, 256, F32, "p (k e) -> p k e", e=NE)
        logT = carve(RB + 512, 1024, F32)
        L = carve(RB + 1536, 128, F32, "p (a e) -> p a e", e=NE)
        L2 = carve(RB + 1664, 128, F32, "p (a e) -> p a e", e=NE)
        E2 = carve(RB + 1792, 128, F32, "p (a e) -> p a e", e=NE)
        m1 = carve(RB + 1920, 16, F32)
        m2 = carve(RB + 1936, 16, F32)
        dd = carve(RB + 1952, 16, F32)
        g1 = carve(RB + 1968, 16, F32)
        g2 = carve(RB + 1984, 16, F32)
        cst = carve(RB + 2000, 2, F32)
        Wt = msm[:, 0:64].rearrange("p (a e) -> p a e", e=NE)
        Wtf = msm[:, 0:64]
        Mx = msm[:, 64:128]
        Mx3 = msm[:, 64:128].rearrange("p (a e) -> p a e", e=NE)
        t1 = msm[:, 128:192]
        rk = msm[:, 192:256]
        rkp = msm[:, 256:512].rearrange("p (q a e) -> p q a e", q=4, e=NE)
        h_tm = carve(0, 16384, BF16, "p (a f) -> p a f", f=D)
        SS = 512
        Sel = carve(16384, 4096, BF16, "p (a s) -> p a s", s=SS)
        SelT = [carve(20480 + st * 1024, 1024, BF16) for st in range(4)]
        hflat = hT[:].rearrange("p k t -> p (k t)")
        hsel = hflat[:, 0:8192].rearrange("p (k s) -> p k s", s=SS)
        ybf = hflat[:, 8192:16384].rearrange("p (a f) -> p a f", f=D)
        actm = ws[2][:, 0:6144].rearrange("p (c s) -> p c s", s=SS)
        iota = rstd[:, 0:512]
        hkeys = [("h", kc, tt) for kc in range(KC) for tt in range(2)]
        rkeys = ["Rr", "cst", "logT", "L", "dd", "g1", "g2"] + [("RA", kc) for kc in range(KC)] + \
                [(nm, a) for nm in ("m1", "m2", "L2", "E2") for a in range(8)]
        nkeys = []

        def setup(_):
            sp_dma(Rr, rt_d, (), ("Rr",), f"rt{i}")
            for kc in range(KC):
                dve(lambda e, kc=kc: e.tensor_scalar(out=RA[:, kc, :], in0=Rr[:, kc, :], scalar1=ABc(3, kc), scalar2=None, op0=ALU.mult),
                    ("Rr", ("AB", 3)), (("RA", kc),))
            for kc in range(KC):
                mm(ps[5][0:8, 0:1], Rr[:, kc, :], ABc(4, kc), kc == 0, kc == KC - 1, ("Rr", ("AB", 4)), (PS(5),))
            dve(lambda e: e.tensor_copy(out=cst[0:8, :], in_=ps[5][0:8, 0:1]), (PS(5),), ("cst",))
        task(None, setup)

        def extra(tt):
            for kc in range(KC):
                mm(ps[5][0:8, :], RA[:, kc, :], xT[:, kc, tt * 512:(tt + 1) * 512], kc == 0, kc == KC - 1,
                   (("RA", kc), ("x", kc, tt)), (PS(5),))
            dve(lambda e: e.tensor_tensor(out=logT[0:8, :], in0=ps[5][0:8, :], in1=rstd[0:8, :], op=ALU.mult), (PS(5), "rstd"), ("logT",))
            dve(lambda e: e.tensor_scalar(out=logT[0:8, :], in0=logT[0:8, :], scalar1=cst[0:8, 0:1], scalar2=None, op0=ALU.add),
                ("logT", "cst"), ("logT",))
            for q in range(4):
                tile = tt * 4 + q
                P.add("pe", (lambda e, tile=tile, q=q: e.transpose(ps[4][:, tile * 8:(tile + 1) * 8], logT[0:8, q * 128:(q + 1) * 128], ident[0:8, 0:8])),
                      reads=("logT", "ident"), writes=(PS(4),))
        norm_mod(1, extra)

        def gates(_):
            sp_dma(iota, iota_d, (), ("rstd",), f"iota{i}")
            dve(lambda e: e.tensor_copy(out=L, in_=ps[4][:, 0:64].rearrange("p (a e) -> p a e", e=NE)), (PS(4),), ("L",))
            for a in range(8):
                dve(lambda e, a=a: e.tensor_reduce(out=m1[:, a:a + 1], in_=L[:, a, :], axis=AX.X, op=ALU.max), ("L",), (("m1", a),))
                dve(lambda e, a=a: e.tensor_scalar(out=L2[:, a, :], in0=L[:, a, :], scalar1=m1[:, a:a + 1], scalar2=-1e30, op0=ALU.is_equal, op1=ALU.mult),
                    ("L", ("m1", a)), (("L2", a),))
                dve(lambda e, a=a: e.tensor_tensor(out=L2[:, a, :], in0=L2[:, a, :], in1=L[:, a, :], op=ALU.add), ("L", ("L2", a)), (("L2", a),))
                dve(lambda e, a=a: e.tensor_reduce(out=m2[:, a:a + 1], in_=L2[:, a, :], axis=AX.X, op=ALU.max), (("L2", a),), (("m2", a),))
            rm = tuple(("m1", a) for a in range(8)) + tuple(("m2", a) for a in range(8))
            dve(lambda e: e.tensor_tensor(out=dd[:, 0:8], in0=m1[:, 0:8], in1=m2[:, 0:8], op=ALU.subtract), rm, ("dd",))
            act(g1[:, 0:8], dd[:, 0:8], AF.Sigmoid, ("dd",), ("g1",))
            act(g2[:, 0:8], dd[:, 0:8], AF.Sigmoid, ("dd",), ("g2",), scale=-1.0)
            for a in range(8):
                dve(lambda e, a=a: e.tensor_scalar(out=Wt[:, a, :], in0=L[:, a, :], scalar1=m1[:, a:a + 1], scalar2=g1[:, a:a + 1], op0=ALU.is_equal, op1=ALU.mult),
                    ("L", ("m1", a), "g1"), (("Wt", a),))
                dve(lambda e, a=a: e.tensor_scalar(out=E2[:, a, :], in0=L2[:, a, :], scalar1=m2[:, a:a + 1], scalar2=g2[:, a:a + 1], op0=ALU.is_equal, op1=ALU.mult),
                    (("L2", a), ("m2", a), "g2"), (("E2", a),))
                dve(lambda e, a=a: e.tensor_tensor(out=Wt[:, a, :], in0=Wt[:, a, :], in1=E2[:, a, :], op=ALU.add), (("Wt", a), ("E2", a)), (("Wt", a),))
            wtk = tuple(("Wt", a) for a in range(8))
            dve(lambda e: e.tensor_single_scalar(out=Mx, in_=Wtf, scalar=0.0, op=ALU.is_gt), wtk, ("Mx",))
            for a in range(8):
                for a2 in range(a):
                    mm(ps[3][:, a * 8:(a + 1) * 8], ones[:], Mx3[:, a2, :], a2 == 0, False, ("Mx", "ones"), (PS(3),))
                mm(ps[3][:, a * 8:(a + 1) * 8], lt[:], Mx3[:, a, :], a == 0, True, ("Mx", "lt"), (PS(3),))
            for a in range(8):
                mm(ps[2][0:1, 0:8], ones[:, 0:1], Mx3[:, a, :], a == 0, a == 7, ("Mx", "ones"), (PS(2),))
            dve(lambda e: e.tensor_copy(out=cntf[:], in_=ps[2][0:1, 0:8]), (PS(2),), ("cntf",))
            for q in range(2):
                dve(lambda e, q=q: e.tensor_single_scalar(out=flags_i[0:1, q * 8:(q + 1) * 8], in_=cntf[:], scalar=512.0 * q + 0.5, op=ALU.is_gt),
                    ("cntf",), (fkey,))
            dve(lambda e: e.tensor_scalar(out=t1, in0=Mx, scalar1=-1.0, scalar2=1e9, op0=ALU.add, op1=ALU.mult), ("Mx",), ("t1",))
            dve(lambda e: e.tensor_tensor(out=rk, in0=ps[3][:, 0:64], in1=Mx, op=ALU.mult), (PS(3), "Mx"), ("rk",))
            dve(lambda e: e.tensor_tensor(out=rk, in0=rk, in1=t1, op=ALU.add), ("rk", "t1"), ("rk",))
            dve(lambda e: e.tensor_copy(out=wtb[:], in_=Wtf), wtk, ("wtb",))
            for q in range(2):
                dve(lambda e, q=q: e.tensor_scalar(out=msm[:, 256 + q * 64:256 + (q + 1) * 64], in0=rk, scalar1=-512.0 * q, scalar2=None, op0=ALU.add),
                    ("rk",), (("rkp", q),))
            n_ = 0
            for a in range(8):
                for half in range(2):
                    b = n_ % 4
                    psb = ps[b][:].bitcast(BF16)
                    for q in range(8):
                        kc = half * 8 + q
                        P.add("pe", (lambda e, psb=psb, q=q, kc=kc, a=a: e.transpose(psb[:, q * 128:(q + 1) * 128], hT[:, kc, a * 128:(a + 1) * 128], identb[:])),
                              reads=(("h", kc, a // 4), "identb"), writes=(PS(b),))
                    dst = h_tm[:, a, half * 1024:(half + 1) * 1024]
                    if n_ % 2 == 0:
                        act(dst, psb, AF.Copy, (PS(b),), (("htm", a),))
                    else:
                        dve(lambda e, dst=dst, psb=psb: e.tensor_copy(out=dst, in_=psb), (PS(b),), (("htm", a),))
                    n_ += 1
            fence(hkeys + rkeys + nkeys)
        task(None, gates)

        rot = [0]
        wtb3 = wtb[:].rearrange("p (a e) -> p a e", e=NE)
        for e_ in range(NE):
            for q in range(2):
                tasks.append(("rb", flags_i[0:1, q * 8 + e_:q * 8 + e_ + 1], fkey))

                def pre(_, e_=e_, q=q):
                    for a in range(8):
                        dve(lambda e, a=a: e.tensor_scalar(out=Sel[:, a, :], in0=iota, scalar1=rkp[:, q, a, e_:e_ + 1], scalar2=None, op0=ALU.is_equal),
                            ("rstd", ("rkp", q)), (("Sel", a),))
                    for st in range(4):
                        for a in range(8):
                            mm(ps[7][:, st:st + 1], Sel[:, a, st * 128:(st + 1) * 128], wtb3[:, a, e_:e_ + 1], a == 0, a == 7,
                               (("Sel", a), "wtb"), (PS(7),))
                    dve(lambda e: e.tensor_copy(out=gslot[:], in_=ps[7][:, 0:4]), (PS(7),), ("gslot",))
                    for kc in range(KC):
                        b = kc % 4
                        for a in range(8):
                            mm(ps[b][:], h_tm[:, a, kc * 128:(kc + 1) * 128], Sel[:, a, :], a == 0, a == 7,
                               (("htm", a), ("Sel", a)), (PS(b),))
                        dst = hsel[:, kc, :]
                        if kc % 2 == 0:
                            act(dst, ps[b][:], AF.Copy, (PS(b),), (("hsel", kc),))
                        else:
                            dve(lambda e, dst=dst, b=b: e.tensor_copy(out=dst, in_=ps[b][:]), (PS(b),), (("hsel", kc),))
                    for st in range(4):
                        b = 4 + st % 2
                        psb = ps[b][:].bitcast(BF16)
                        for a in range(8):
                            P.add("pe", (lambda e, psb=psb, a=a, st=st: e.transpose(psb[:, a * 128:(a + 1) * 128], Sel[:, a, st * 128:(st + 1) * 128], identb[:])),
                                  reads=(("Sel", a), "identb"), writes=(PS(b),))
                        if st % 2 == 0:
                            act(SelT[st], psb, AF.Copy, (PS(b),), (("SelT", st),))
                        else:
                            dve(lambda e, psb=psb, st=st: e.tensor_copy(out=SelT[st], in_=psb), (PS(b),), (("SelT", st),))
                task(None, pre)

                w13, w2 = mw13[e_], mw2[e_]
                for (c0, n) in SECTIONS:
                    for blk in range(n // 2):
                        c = c0 + 2 * blk

                        def fn(s, c=c, c0=c0):
                            wv = wview(s, KC, 512)
                            for ci in range(2):
                                r = rot[0] % 3
                                rot[0] += 1
                                ba, bb = 2 * r, 2 * r + 1
                                for kc in range(KC):
                                    mm(ps[ba][:], wv[:, kc, ci * 128:(ci + 1) * 128], hsel[:, kc, :], kc == 0, kc == KC - 1,
                                       (("w", s, 0), ("w", s, 1), ("hsel", kc)), (PS(ba),))
                                    mm(ps[bb][:], wv[:, kc, 256 + ci * 128:256 + (ci + 1) * 128], hsel[:, kc, :], kc == 0, kc == KC - 1,
                                       (("w", s, 0), ("w", s, 1), ("hsel", kc)), (PS(bb),))
                                sb_ = r % 2
                                act(sa[sb_][:], ps[ba][:], AF.Silu, (PS(ba),), (("sa", sb_),))
                                dst = actm[:, c + ci - c0, :]
                                dve(lambda e, dst=dst, sb_=sb_, bb=bb: e.tensor_tensor(out=dst, in0=ps[bb][:], in1=sa[sb_][:], op=ALU.mult),
                                    (PS(bb), ("sa", sb_)), (("actm", c + ci - c0),))
                        task([(KC, 512, 0, 256, wsrc(w13, 0, KC, c * 128, 256)),
                              (KC, 512, 256, 256, wsrc(w13, 0, KC, DFF + c * 128, 256))], fn)
                    for nb in range(4):
                        def fn2(s, nb=nb, n=n):
                            wv = wview(s, n, 512)
                            for st in range(4):
                                b = 6 + dn_rot[0] % 2
                                dn_rot[0] += 1
                                for k in range(n):
                                    mm(ps[b][:], actm[:, k, st * 128:(st + 1) * 128], wv[:, k, :], k == 0, k == n - 1,
                                       (("w", s, 0), ("w", s, 1), ("actm", k)), (PS(b),))
                                dst = ybf[:, st, nb * 512:(nb + 1) * 512]
                                if st % 2 == 0:
                                    act(dst, ps[b][:], AF.Copy, (PS(b), "gslot"), (("ybf", st, nb),), scale=gslot[:, st:st + 1])
                                else:
                                    dve(lambda e, dst=dst, b=b, st=st: e.tensor_scalar(out=dst, in0=ps[b][:], scalar1=gslot[:, st:st + 1], scalar2=None, op0=ALU.mult),
                                        (PS(b), "gslot"), (("ybf", st, nb),))
                        task([(n, 512, 0, 512, wsrc(w2, c0 * 128, n, nb * 512, 512))], fn2)

                    def comb(_):
                        for c in range(KC):
                            for tt in range(2):
                                b = rot[0] % 3 * 2 + (rot[0] // 3) % 2
                                rot[0] += 1
                                for st in range(4):
                                    mm(ps[b][:], ybf[:, st, c * 128:(c + 1) * 128], SelT[st][:, tt * 512:(tt + 1) * 512], st == 0, st == 3,
                                       (("ybf", st, c // 4), ("SelT", st)), (PS(b),))
                                xs = xT[:, c, tt * 512:(tt + 1) * 512]
                                dve(lambda e, xs=xs, b=b, c=c: e.scalar_tensor_tensor(out=xs, in0=ps[b][:], scalar=ABc(5, c), in1=xs, op0=ALU.mult, op1=ALU.add),
                                    (PS(b), ("x", c, tt), ("AB", 5)), (("x", c, tt),))
                    task(None, comb)
                tasks.append(("re",))

        def fin(_):
            fence(hkeys + rkeys + nkeys)
        task(None, fin)

    def store(_):
        for tt in range(2):
            if final_norm:
                norm_stats(tt, 6)
            for kc in range(KC):
                b = kc % 2
                src = xT[:, kc, tt * 512:(tt + 1) * 512]
                if final_norm:
                    dve(lambda e, b=b, kc=kc, src=src: e.scalar_tensor_tensor(out=nt[b][:], in0=src, scalar=fgT[:, kc:kc + 1], in1=rstd[:],
                                                                              op0=ALU.mult, op1=ALU.mult),
                        (("x", kc, tt), "rstd", "fgT"), (("nt", b),))
                    src = nt[b][:]
                    rd = (("nt", b),)
                else:
                    rd = (("x", kc, tt),)
                sp_dma(out_d[kc * 128:(kc + 1) * 128, tt * 512:(tt + 1) * 512], src, rd, (("out", kc, tt),), f"o{b}")
        P.add("sp", (lambda e: e.nop()), reads=tuple(("out", kc, tt) for kc in range(KC) for tt in range(2)), writes=())

    for i in layers:
        ada_layer(i)
    for kind, i in plan:
        task(None, lambda _: fence())
        prep_AB(i)
        if kind == "mixer":
            norm_mod(0)
            if i % 2 == 0:
                conv_mixer(i)
            else:
                gla_mixer(i)
        else:
            if i % 2 == 0:
                norm_mod(1)
                ffn(W[("w13", i)], W[("w2", i)], 5)
            else:
                moe(i)
    task(None, store)
    run_tasks()
    P.emit()
    es.close()
    return nc, P


def _consts():
    ident = np.eye(128, dtype=np.float32)
    ones = np.ones((128, 128), np.float32)
    s = np.arange(128)[:, None]
    t = np.arange(128)[None, :]
    u2 = (((s // 64) == (t // 64)) & (s > t)).astype(np.float32) * (-1.0 / 16.0)
    ind2 = ((s // 64) == np.arange(2)[None, :]).astype(np.float32) * (-1.0 / 16.0)
    lt = (s < t).astype(np.float32)
    iota = np.ascontiguousarray(np.broadcast_to(np.arange(512, dtype=np.float32)[None, :], (128, 512)))
    return ident, ones, u2, ind2, lt, iota


def make_in_maps(plan, xs, inputs):
    ident, ones, u2, ind2, lt, iota = _consts()
    layers = sorted({i for _, i in plan})
    c = inputs["c"]
    ada_b = np.ascontiguousarray(inputs["ada_b"].reshape(4, 96, 128).transpose(0, 2, 1))
    ng = np.ascontiguousarray(inputs["norm_g"].reshape(4, 2, 16, 128).transpose(3, 0, 1, 2).reshape(128, 128))
    fg = np.ascontiguousarray(inputs["final_g"].reshape(16, 128).T)
    ck = np.ascontiguousarray(inputs["conv_k"].reshape(2, 3, 16, 128).transpose(3, 0, 1, 2).reshape(128, 96))
    shared = {"ada_bT": ada_b, "norm_gT": ng, "final_gT": fg, "conv_kT": ck, "ident": ident, "ones": ones, "u2": u2, "ind2": ind2, "lt": lt, "iota": iota}
    for i in layers:
        shared[f"ada_w{i}"] = inputs["ada_w"][i]
    for kind, i in plan:
        j = i // 2
        if kind == "mixer" and i % 2 == 0:
            shared[f"conv_w_in{j}"] = inputs["conv_w_in"][j]
            shared[f"conv_w_out{j}"] = inputs["conv_w_out"][j]
        if kind == "mixer" and i % 2 == 1:
            shared[f"gla_w_in{j}"] = inputs["gla_w_in"][j]
            shared[f"gla_w_out{j}"] = inputs["gla_w_out"][j]
            shared[f"gla_wgk{j}"] = np.concatenate([inputs["gla_w_gk"][j], inputs["gla_b_gk"][j][None, :]], 0)
            shared[f"gla_ng{j}"] = np.ascontiguousarray(np.broadcast_to(inputs["gla_norm_g"][j][None, :], (128, 512)))
        if kind == "ffn" and i % 2 == 0:
            shared[f"ffn_w13_{j}"] = inputs["ffn_w13"][j]
            shared[f"ffn_w2_{j}"] = inputs["ffn_w2"][j]
        if kind == "ffn" and i % 2 == 1:
            shared[f"moe_router{j}"] = np.ascontiguousarray(inputs["moe_router"][j].reshape(16, 128, 8).transpose(1, 0, 2))
            shared[f"moe_w13_{j}"] = inputs["moe_w13"][j]
            shared[f"moe_w2_{j}"] = inputs["moe_w2"][j]
    in_maps = []
    for r in range(8):
        b, hf = r // 2, r % 2
        m = dict(shared)
        m["xT"] = np.ascontiguousarray(xs[r].T)
        m["cT"] = np.ascontiguousarray(c[b].reshape(16, 128).T)
        m["flag"] = np.full((128, 1), float(hf), np.float32)
        in_maps.append(m)
    return in_maps


def run_plan(plan, xs, inputs, final_norm, trace=False):
    nc, P = build_program(plan, final_norm)
    in_maps = make_in_maps(plan, xs, inputs)
    res = run_bass_kernel_spmd(nc, in_maps, core_ids=list(range(8)), trace=trace)
    outs = [np.ascontiguousarray(res.results[r]["outT"].T) for r in range(8)]
    return outs, res


def shard_x(x):
    return [np.ascontiguousarray(x[r // 2, (r % 2) * T:(r % 2 + 1) * T, :]) for r in range(8)]


def unshard_x(outs):
    full = np.zeros((4, 2 * T, D), np.float32)
    for r in range(8):
        full[r // 2, (r % 2) * T:(r % 2 + 1) * T, :] = outs[r]
    return full


FULL_PLAN = [("mixer", 0), ("ffn", 0), ("mixer", 1), ("ffn", 1), ("mixer", 2), ("ffn", 2), ("mixer", 3), ("ffn", 3)]


def kernel(**inputs):
    inputs = {k: np.asarray(v) for k, v in inputs.items()}
    xs = shard_x(inputs["x"])
    outs, _ = run_plan(FULL_PLAN, xs, inputs, final_norm=True)
    return unshard_x(outs)
```

```python
import contextlib
import numpy as np
import concourse.bass as bass
import concourse.mybir as mybir
from concourse.bass_utils import run_bass_kernel_spmd

F32 = mybir.dt.float32
BF16 = mybir.dt.bfloat16
AF = mybir.ActivationFunctionType
ALU = mybir.AluOpType
AX = mybir.AxisListType

D = 2048
T = 1024
KC = 16
DFF = 5632
NE = 8
EPS = 1e-6
SEM_ROT = 20000
PAIRS = [[0, 1], [2, 3], [4, 5], [6, 7]]


class Prog:
    def __init__(self, nc, es):
        self.nc = nc
        self.es = es
        self.ins = []
        self.last_w = {}
        self.readers = {}
        self.dsems = {}

    def add(self, eng, fn, reads=(), writes=(), dsem=None, inc=16, mark=None, nofence=False):
        k = len(self.ins)
        deps = set()
        if not nofence and "EPOCH" not in writes:
            reads = tuple(reads) + ("EPOCH",)
        for r in reads:
            w = self.last_w.get(r)
            if w is not None:
                deps.add(w)
        for w_ in writes:
            w = self.last_w.get(w_)
            if w is not None:
                deps.add(w)
            deps |= self.readers.get(w_, set())
        if mark is None:
            for r in reads:
                self.readers.setdefault(r, set()).add(k)
        for w_ in writes:
            self.last_w[w_] = k
            self.readers[w_] = set()
        deps.discard(k)
        self.ins.append(dict(eng=eng, fn=fn, deps=deps, dsem=dsem, inc=inc, mark=mark))
        return k

    def region_begin(self, flag_ap, flagkey):
        for eng in ("pe", "act", "dve", "pool", "sp"):
            self.add(eng, None, reads=(flagkey,), mark=("rb", flag_ap))

    def region_end(self):
        for eng in ("pe", "act", "dve", "pool", "sp"):
            self.add(eng, None, mark=("re",))

    def emit(self):
        nc = self.nc
        ins = self.ins
        n = len(ins)
        needs = [False] * n
        for k, it in enumerate(ins):
            for d in it["deps"]:
                if ins[d]["dsem"] is None and not (ins[d]["eng"] == "pe" and it["eng"] == "pe"):
                    needs[d] = True
        sig = [None] * n
        cnt = {}
        semobjs = {}

        def getsem(name):
            if name not in semobjs:
                semobjs[name] = self.es.enter_context(nc.semaphore(name))
            return semobjs[name]

        dcnt = {}
        for k, it in enumerate(ins):
            if it["dsem"] is not None:
                dcnt[it["dsem"]] = dcnt.get(it["dsem"], 0) + it["inc"]
                sig[k] = (getsem("d_" + it["dsem"]), dcnt[it["dsem"]])
            elif needs[k]:
                e = it["eng"]
                c = cnt.get(e, 0)
                cnt[e] = c + 1
                sig[k] = (getsem(f"c_{e}_{c // SEM_ROT}"), c % SEM_ROT + 1)
        streams = {}
        for k, it in enumerate(ins):
            streams.setdefault(it["eng"], []).append(k)
        self.stats = {e: len(v) for e, v in streams.items()}
        block = self.es.enter_context(nc.Block())

        def run_stream(eng_name, eobj):
            waited = {}
            st_ = streams.get(eng_name, [])
            reg = None
            for pos, k in enumerate(st_):
                it = ins[k]
                if it["mark"] is not None and it["mark"][0] == "re":
                    if reg is None:
                        continue
                    reg["guard"].__exit__(None, None, None)
                    comp = {}
                    for kk in reg["members"]:
                        if sig[kk] is not None:
                            s_, v_ = sig[kk]
                            inc_ = ins[kk]["inc"] if ins[kk]["dsem"] is not None else 1
                            c_ = comp.setdefault(id(s_), [s_, v_ - inc_, 0, ins[kk]["dsem"] is not None])
                            c_[2] += inc_
                    if comp:
                        with eobj.Else():
                            if any(not c_[3] for c_ in comp.values()):
                                eobj.drain()
                            for s_, before, tot, isd in comp.values():
                                if isd and before > 0:
                                    eobj.wait_ge(s_, before)
                                eobj.sem_inc(s_, tot)
                    waited = reg["waited"]
                    reg["rctx"].__exit__(None, None, None)
                    reg = None
                    continue
                if it["mark"] is not None and it["mark"][0] == "rb":
                    members = []
                    for k2 in st_[pos + 1:]:
                        if ins[k2]["mark"] is not None:
                            break
                        members.append(k2)
                    if not members:
                        continue
                want = {}
                for d in it["deps"]:
                    if ins[d]["dsem"] is None and ins[d]["eng"] == "pe" and eng_name == "pe":
                        continue
                    s, v = sig[d]
                    key = id(s)
                    if key not in want or want[key][1] < v:
                        want[key] = (s, v)
                for key, (s, v) in want.items():
                    if waited.get(key, 0) < v:
                        eobj.wait_ge(s, v)
                        waited[key] = v
                if it["mark"] is not None:
                    rctx = eobj.register(f"rf_{eng_name}_{k}")
                    r_ = rctx.__enter__()
                    eobj.reg_load(r_, it["mark"][1])
                    guard = eobj.If_ne(r_, 0)
                    guard.__enter__()
                    reg = dict(guard=guard, members=members, waited=dict(waited), rctx=rctx)
                    continue
                bi = it["fn"](eobj)
                if sig[k] is not None:
                    if it["dsem"] is not None:
                        bi.then_inc(sig[k][0], it["inc"])
                    else:
                        bi.then_inc(sig[k][0], 1)

        @block.tensor
        def _(e):
            run_stream("pe", e)

        @block.scalar
        def _(e):
            run_stream("act", e)

        @block.vector
        def _(e):
            run_stream("dve", e)

        @block.gpsimd
        def _(e):
            run_stream("pool", e)

        @block.sync
        def _(e):
            run_stream("sp", e)


def build_program(plan, final_norm, out_mode="x"):
    nc = bass.Bass("TRN2", target_bir_lowering=False)
    es = contextlib.ExitStack()
    P = Prog(nc, es)
    layers = sorted({i for _, i in plan})

    def din(name, shape, dt=F32):
        return nc.dram_tensor(name, list(shape), dt, kind="ExternalInput").ap()

    x_d = din("xT", [D, T])
    out_d = nc.dram_tensor("outT", [D, T], F32, kind="ExternalOutput").ap()
    cT_d = din("cT", [128, KC])
    adab_d = din("ada_bT", [4, 128, 96])
    ng_d = din("norm_gT", [128, 128])
    fg_d = din("final_gT", [128, KC])
    ck_d = din("conv_kT", [128, 96])
    ident_d = din("ident", [128, 128])
    ones_d = din("ones", [128, 128])
    u2_d = din("u2", [128, 128])
    ind2_d = din("ind2", [128, 2])
    flag_d = din("flag", [128, 1])
    lt_d = din("lt", [128, 128])
    iota_d = din("iota", [128, 512])
    W = {}
    for i in layers:
        j = i // 2
        W[("ada", i)] = din(f"ada_w{i}", [D, 6 * D])
    for kind, i in plan:
        j = i // 2
        if kind == "mixer" and i % 2 == 0:
            W[("cin", i)] = din(f"conv_w_in{j}", [D, 3 * D])
            W[("cout", i)] = din(f"conv_w_out{j}", [D, D])
        if kind == "mixer" and i % 2 == 1:
            W[("gin", i)] = din(f"gla_w_in{j}", [D, 6160])
            W[("gout", i)] = din(f"gla_w_out{j}", [D, D])
            W[("wgk", i)] = din(f"gla_wgk{j}", [17, 1024])
            W[("gng", i)] = din(f"gla_ng{j}", [128, 512])
        if kind == "ffn" and i % 2 == 0:
            W[("w13", i)] = din(f"ffn_w13_{j}", [D, 2 * DFF])
            W[("w2", i)] = din(f"ffn_w2_{j}", [DFF, D])
        if kind == "ffn" and i % 2 == 1:
            W[("rt", i)] = din(f"moe_router{j}", [128, KC, NE])
            W[("mw13", i)] = din(f"moe_w13_{j}", [NE, D, 2 * DFF])
            W[("mw2", i)] = din(f"moe_w2_{j}", [NE, DFF, D])
    cc_h_in = nc.dram_tensor("cc_h_in", [128, 32], BF16)
    cc_h_out = nc.dram_tensor("cc_h_out", [256, 32], BF16)
    cc_s_in = nc.dram_tensor("cc_s_in", [128, 1024], F32)
    cc_s_out = nc.dram_tensor("cc_s_out", [256, 1024], F32)

    def sb(name, shape, dt):
        return es.enter_context(nc.sbuf_tensor(name, list(shape), dt))

    xT = sb("xT_s", [128, KC, T], F32)
    hT = sb("hT_s", [128, KC, T], BF16)
    ws = [sb(f"ws{i}", [128, 8192], BF16) for i in range(3)]
    ARENA = 24640
    arena = sb("arena", [128, ARENA], BF16)
    ident = sb("ident_s", [128, 128], F32)
    ones = sb("ones_s", [128, 128], F32)
    u2 = sb("u2_s", [128, 128], F32)
    ind2 = sb("ind2_s", [128, 2], F32)
    flag = sb("flag_s", [128, 1], F32)
    lt = sb("lt_s", [128, 128], F32)
    identb = sb("identb", [128, 128], BF16)
    msm = sb("msm", [128, 512], F32)
    wtb = sb("wtb", [128, 64], BF16)
    gslot = sb("gslot", [128, 4], F32)
    flags_all = [sb(f"flags_i{q}", [1, 32], mybir.dt.int32) for q in range(2)]
    cntf = sb("cntf", [1, 8], F32)
    dummy = sb("dummy_f", [1, 8], F32)
    modT = sb("modT", [128, 4 * 96], F32)
    ngT = sb("ngT", [128, 128], F32)
    fgT = sb("fgT", [128, KC], F32)
    ckT = sb("ckT", [128, 96], F32)
    craw = sb("craw", [128, KC], F32)
    cact = sb("cact", [128, KC], BF16)
    AB = sb("AB", [128, 6 * KC], F32)
    nt = [sb(f"nt{i}", [128, 512], F32) for i in range(2)]
    rstd = sb("rstd", [128, 512], F32)
    sa = [sb(f"sa{i}", [128, 512], BF16) for i in range(2)]
    ps = [es.enter_context(nc.psum_tensor(f"ps{i}", [128, 512], F32)) for i in range(8)]

    def PS(b):
        return ("ps", b)

    modrow = [arena[0:1, i * 1024:(i + 1) * 1024].bitcast(F32) for i in range(2)]
    adab = arena[:, 4096:4096 + 768].bitcast(F32)

    def carve(off, n_bf16, dt, pattern=None, **kw):
        v = arena[:, off:off + n_bf16]
        if dt == F32:
            v = v.bitcast(F32)
        if pattern:
            v = v.rearrange(pattern, **kw)
        return v

    tasks = []

    def task(dmas, fn):
        tasks.append((dmas, fn))

    def wview(s, nk, w):
        return ws[s][:, 0:nk * w].rearrange("p (k w) -> p k w", w=w)

    def run_tasks():
        widx = [i for i, t_ in enumerate(tasks) if t_[0] not in ("rb", "re") and t_[0] is not None]
        slot_of = {ti: n % 2 for n, ti in enumerate(widx)}
        wpos = {ti: n for n, ti in enumerate(widx)}
        issued = [0]

        def issue_upto(nw, limit_ti):
            while issued[0] < min(nw, len(widx)) and widx[issued[0]] < limit_ti:
                ti = widx[issued[0]]
                s = slot_of[ti]
                dl = tasks[ti][0]
                for qi, (nk, w, off, ncols, src) in enumerate(dl):
                    dst = wview(s, nk, w)[:, :, off:off + ncols]
                    wk = (("w", s, qi),) if len(dl) == 2 else (("w", s, 0), ("w", s, 1))
                    P.add("pool", (lambda e, dst=dst, src=src: e.dma_start(out=dst, in_=src)),
                          reads=(), writes=wk, dsem=f"w{s}", nofence=True)
                issued[0] += 1

        marks = [i for i, t_ in enumerate(tasks) if t_[0] in ("rb", "re")] + [len(tasks)]
        nw_done = 0
        for ti, t_ in enumerate(tasks):
            if t_[0] == "rb":
                P.region_begin(t_[1], t_[2])
                continue
            if t_[0] == "re":
                P.region_end()
                continue
            limit = min(m for m in marks if m > ti)
            dmas, fn = t_
            if dmas is not None:
                issue_upto(nw_done + 2, limit)
                fn(slot_of[ti])
                nw_done += 1
            else:
                issue_upto(nw_done + 1, limit)
                fn(None)

    def wsrc(w_ap, r0, nk, c0, ncols):
        return w_ap[r0:r0 + nk * 128, c0:c0 + ncols].rearrange("(k p) n -> p k n", p=128)

    def mm(out, lhsT, rhs, start, stop, reads, writes):
        P.add("pe", (lambda e: e.matmul(out, lhsT, rhs, start=start, stop=stop)), reads=reads, writes=writes)

    def act(out, in_, func, reads, writes, **kw):
        P.add("act", (lambda e: e.activation(out=out, in_=in_, func=func, **kw)), reads=reads, writes=writes)

    def dve(fn, reads, writes):
        P.add("dve", fn, reads=reads, writes=writes)

    def sp_dma(out, in_, reads, writes, dsem):
        P.add("sp", (lambda e: e.dma_start(out=out, in_=in_)), reads=reads, writes=writes, dsem=dsem)

    def load_consts(_):
        for dst, src, key in [(ident, ident_d, "ident"), (ones, ones_d, "ones"), (u2, u2_d, "u2"),
                              (ind2, ind2_d, "ind2"), (flag, flag_d, "flag"), (ngT, ng_d, "ngT"),
                              (fgT, fg_d, "fgT"), (ckT, ck_d, "ckT"), (craw, cT_d, "craw"), (lt, lt_d, "lt")]:
            sp_dma(dst[:], src, (), (key,), "c_" + key)
        for i in layers:
            sp_dma(adab[:, i * 96:(i + 1) * 96], adab_d[i], (), (("adab", i),), f"c_adab{i}")
        for kc in range(KC):
            sp_dma(xT[:, kc, :], x_d[kc * 128:(kc + 1) * 128, :], (), tuple(("x", kc, tt) for tt in range(2)), f"x{kc}")
        act(cact[:], craw[:], AF.Silu, ("craw",), ("cact",))
        P.add("pool", (lambda e: e.dma_start(out=identb[:], in_=ident_d)), reads=(), writes=("identb",), dsem="c_identb")

    task(None, load_consts)

    def ada_layer(i):
        wa = W[("ada", i)]
        PSB = 7

        def blk(nb):
            def fn(s):
                wv = wview(s, KC, 512)
                b = nb % 2
                for kc in range(KC):
                    mm(ps[b][0:1, :], cact[:, kc:kc + 1], wv[:, kc, :], kc == 0, kc == KC - 1,
                       (("w", s, 0), ("w", s, 1), "cact"), (PS(b),))
                mr = modrow[nb % 2]
                act(mr, ps[b][0:1, :], AF.Copy, (PS(b),), (("modrow", nb % 2),))
                for q in range(4):
                    col = nb * 4 + q
                    mm(ps[PSB][:, col:col + 1], mr[0:1, q * 128:(q + 1) * 128], ones[0:1, 0:1], True, True,
                       (("modrow", nb % 2), "ones"), (PS(PSB),))
                if nb == 23:
                    dve(lambda e: e.tensor_tensor(out=modT[:, i * 96:(i + 1) * 96], in0=ps[PSB][:, 0:96],
                                                  in1=adab[:, i * 96:(i + 1) * 96], op=ALU.add),
                        (PS(PSB), ("adab", i)), (("mod", i),))
            return fn

        for nb in range(24):
            task([(KC, 512, 0, 512, wsrc(wa, 0, KC, nb * 512, 512))], blk(nb))

    def m_col(i, sec):
        return modT[:, i * 96 + sec * 16: i * 96 + (sec + 1) * 16]

    def prep_AB(i):
        def fn(_):
            for s_ in range(2):
                ng = ngT[:, i * 32 + s_ * 16: i * 32 + (s_ + 1) * 16]
                sc = m_col(i, 3 * s_ + 1)
                A = AB[:, (3 * s_) * 16:(3 * s_ + 1) * 16]
                dve(lambda e, A=A, sc=sc, ng=ng: e.scalar_tensor_tensor(out=A, in0=sc, scalar=1.0, in1=ng,
                                                                         op0=ALU.add, op1=ALU.mult),
                    (("mod", i), "ngT"), (("AB", 3 * s_),))
                B = AB[:, (3 * s_ + 1) * 16:(3 * s_ + 2) * 16]
                dve(lambda e, B=B, sh=m_col(i, 3 * s_): e.tensor_copy(out=B, in_=sh),
                    (("mod", i),), (("AB", 3 * s_ + 1),))
                G = AB[:, (3 * s_ + 2) * 16:(3 * s_ + 3) * 16]
                dve(lambda e, G=G, g=m_col(i, 3 * s_ + 2): e.tensor_copy(out=G, in_=g),
                    (("mod", i),), (("AB", 3 * s_ + 2),))
        task(None, fn)

    def ABc(idx, kc):
        return AB[:, idx * 16 + kc: idx * 16 + kc + 1]

    def norm_stats(tt, bank):
        for kc in range(KC):
            b = kc % 2
            act(nt[b][:], xT[:, kc, tt * 512:(tt + 1) * 512], AF.Square, (("x", kc, tt),), (("nt", b),))
            mm(ps[bank][:], ones[:], nt[b][:], kc == 0, kc == KC - 1, (("nt", b), "ones"), (PS(bank),))
        act(rstd[:], ps[bank][:], AF.Sqrt, (PS(bank),), ("rstd",), scale=1.0 / D, bias=EPS)
        dve(lambda e: e.reciprocal(out=rstd[:], in_=rstd[:]), ("rstd",), ("rstd",))

    def norm_mod(s_, extra=None):
        def fn(_):
            for tt in range(2):
                norm_stats(tt, 6)
                for kc in range(KC):
                    b = kc % 2
                    dve(lambda e, b=b, kc=kc, tt=tt: e.scalar_tensor_tensor(
                        out=nt[b][:], in0=xT[:, kc, tt * 512:(tt + 1) * 512], scalar=ABc(3 * s_, kc),
                        in1=rstd[:], op0=ALU.mult, op1=ALU.mult),
                        (("x", kc, tt), "rstd", ("AB", 3 * s_)), (("nt", b),))
                    act(hT[:, kc, tt * 512:(tt + 1) * 512], nt[b][:], AF.Identity,
                        (("nt", b), ("AB", 3 * s_ + 1)), (("h", kc, tt),), bias=ABc(3 * s_ + 1, kc), scale=1.0)
                if extra is not None:
                    extra(tt)
        task(None, fn)

    SECTIONS = [(0, 12), (12, 12), (24, 10), (34, 10)]
    ACT_OFF = 0
    actb = carve(ACT_OFF, 12 * 1024, BF16, "p (c t) -> p c t", t=T)
    WB_OFF = 12288
    Wb = carve(WB_OFF, 8 * 1024, BF16, "p (e t) -> p e t", t=T)
    sa2 = [carve(20480 + i * 512, 512, BF16) for i in range(2)]
    up_rot = [0]
    dn_rot = [0]

    def ffn(w13, w2, gidx, gate_e=None):
        for (c0, n) in SECTIONS:
            for blk in range(n // 2):
                c = c0 + 2 * blk

                def fn(s, c=c, c0=c0):
                    wv = wview(s, KC, 512)
                    for ci in range(2):
                        for tt in range(2):
                            r = up_rot[0] % 3
                            up_rot[0] += 1
                            ba, bb = 2 * r, 2 * r + 1
                            for kc in range(KC):
                                mm(ps[ba][:], wv[:, kc, ci * 128:(ci + 1) * 128], hT[:, kc, tt * 512:(tt + 1) * 512],
                                   kc == 0, kc == KC - 1, (("w", s, 0), ("w", s, 1), ("h", kc, tt)), (PS(ba),))
                                mm(ps[bb][:], wv[:, kc, 256 + ci * 128:256 + (ci + 1) * 128],
                                   hT[:, kc, tt * 512:(tt + 1) * 512],
                                   kc == 0, kc == KC - 1, (("w", s, 0), ("w", s, 1), ("h", kc, tt)), (PS(bb),))
                            sb_ = r % 2
                            act(sa[sb_][:], ps[ba][:], AF.Silu, (PS(ba),), (("sa", sb_),))
                            dst = actb[:, c + ci - c0, tt * 512:(tt + 1) * 512]
                            if gate_e is None:
                                dve(lambda e, dst=dst, sb_=sb_, bb=bb: e.tensor_tensor(out=dst, in0=ps[bb][:], in1=sa[sb_][:], op=ALU.mult),
                                    (PS(bb), ("sa", sb_)), (("act", c + ci - c0, tt),))
                            else:
                                dve(lambda e, sb_=sb_, tt=tt: e.tensor_tensor(out=sa2[sb_], in0=sa[sb_][:], in1=Wb[:, gate_e, tt * 512:(tt + 1) * 512], op=ALU.mult),
                                    (("sa", sb_), ("Wb", gate_e, tt)), (("sa2", sb_),))
                                dve(lambda e, dst=dst, sb_=sb_, bb=bb: e.tensor_tensor(out=dst, in0=ps[bb][:], in1=sa2[sb_], op=ALU.mult),
                                    (PS(bb), ("sa2", sb_)), (("act", c + ci - c0, tt),))
                task([(KC, 512, 0, 256, wsrc(w13, 0, KC, c * 128, 256)),
                      (KC, 512, 256, 256, wsrc(w13, 0, KC, DFF + c * 128, 256))], fn)
            for nb in range(4):
                def fn2(s, nb=nb, n=n):
                    wv = wview(s, n, 512)
                    for nci in range(4):
                        for tt in range(2):
                            b = 6 + dn_rot[0] % 2
                            dn_rot[0] += 1
                            for k in range(n):
                                mm(ps[b][:], wv[:, k, nci * 128:(nci + 1) * 128], actb[:, k, tt * 512:(tt + 1) * 512],
                                   k == 0, k == n - 1, (("w", s, 0), ("w", s, 1), ("act", k, tt)), (PS(b),))
                            kc = nb * 4 + nci
                            xs = xT[:, kc, tt * 512:(tt + 1) * 512]
                            dve(lambda e, xs=xs, b=b, kc=kc: e.scalar_tensor_tensor(out=xs, in0=ps[b][:], scalar=ABc(gidx, kc), in1=xs,
                                                                                    op0=ALU.mult, op1=ALU.add),
                                (PS(b), ("x", kc, tt), ("AB", gidx)), (("x", kc, tt),))
                task([(n, 512, 0, 512, wsrc(w2, c0 * 128, n, nb * 512, 512))], fn2)

    def conv_mixer(i):
        j = i // 2
        win, wout = W[("cin", i)], W[("cout", i)]
        VW = T + 2
        vT = carve(0, KC * VW, BF16, "p (c t) -> p c t", t=VW)
        cg = [carve(16416 + q * 4096, 4096, BF16, "p (c t) -> p c t", t=T) for q in range(2)]
        halo = carve(24608, 32, BF16, "p (c t) -> p c t", t=2)
        ctmp = [nt[0], nt[1]]
        rot = [0]
        for blk in range(8):
            c = 2 * blk

            def fn(s, c=c):
                wv = wview(s, KC, 512)
                for ci in range(2):
                    for tt in range(2):
                        r = rot[0] % 3
                        rot[0] += 1
                        ba, bb = 2 * r, 2 * r + 1
                        for kc in range(KC):
                            mm(ps[ba][:], wv[:, kc, ci * 128:(ci + 1) * 128], hT[:, kc, tt * 512:(tt + 1) * 512],
                               kc == 0, kc == KC - 1, (("w", s, 0), ("w", s, 1), ("h", kc, tt)), (PS(ba),))
                            mm(ps[bb][:], wv[:, kc, 256 + ci * 128:256 + (ci + 1) * 128], hT[:, kc, tt * 512:(tt + 1) * 512],
                               kc == 0, kc == KC - 1, (("w", s, 0), ("w", s, 1), ("h", kc, tt)), (PS(bb),))
                        sb_ = r % 2
                        act(sa[sb_][:], ps[ba][:], AF.Copy, (PS(ba),), (("sa", sb_),))
                        dst = vT[:, c + ci, 2 + tt * 512:2 + (tt + 1) * 512]
                        dve(lambda e, dst=dst, sb_=sb_, bb=bb: e.tensor_tensor(out=dst, in0=ps[bb][:], in1=sa[sb_][:], op=ALU.mult),
                            (PS(bb), ("sa", sb_)), (("v", c + ci, tt),))
            task([(KC, 512, 0, 256, wsrc(win, 0, KC, D + c * 128, 256)),
                  (KC, 512, 256, 256, wsrc(win, 0, KC, 2 * D + c * 128, 256))], fn)

        def exch(_):
            sp_dma(cc_h_in.ap().rearrange("p (c t) -> p c t", t=2), vT[:, :, T:T + 2],
                   tuple(("v", c, 1) for c in range(KC)), ("cc_h_in",), f"hx{i}a")
            P.add("pool", (lambda e: e.collective_compute("AllGather", ALU.bypass, replica_groups=PAIRS,
                                                          ins=[cc_h_in.ap().opt()], outs=[cc_h_out.ap().opt()])),
                  reads=("cc_h_in",), writes=("cc_h_out",), dsem=f"hx{i}b", inc=1)
            sp_dma(halo, cc_h_out.ap()[0:128, :].rearrange("p (c t) -> p c t", t=2), ("cc_h_out",), ("halo",), f"hx{i}c")
            dve(lambda e: e.tensor_scalar(out=vT[:, :, 0:2], in0=halo, scalar1=flag[:, 0:1], scalar2=None, op0=ALU.mult),
                ("halo", "flag"), tuple(("vh", c) for c in range(KC)))
        task(None, exch)

        def ck(tap, kc):
            o = j * 48 + tap * 16 + kc
            return ckT[:, o:o + 1]

        for c4 in range(4):
            def fn(s, c4=c4):
                wv = wview(s, KC, 512)
                for ci in range(4):
                    c = c4 * 4 + ci
                    for tt in range(2):
                        r = rot[0] % 3
                        rot[0] += 1
                        ba = 2 * r
                        for kc in range(KC):
                            mm(ps[ba][:], wv[:, kc, ci * 128:(ci + 1) * 128], hT[:, kc, tt * 512:(tt + 1) * 512],
                               kc == 0, kc == KC - 1, (("w", s, 0), ("w", s, 1), ("h", kc, tt)), (PS(ba),))
                        tb = (2 * ci + tt) % 2
                        tmp = ctmp[tb]
                        rd = (("v", c, tt), ("v", c, 0), ("vh", c), "ckT")
                        o = tt * 512
                        dve(lambda e, tmp=tmp, c=c, o=o: e.tensor_scalar(out=tmp[:], in0=vT[:, c, o + 2:o + 514], scalar1=ck(2, c), scalar2=None, op0=ALU.mult),
                            rd, (("nt", tb),))
                        dve(lambda e, tmp=tmp, c=c, o=o: e.scalar_tensor_tensor(out=tmp[:], in0=vT[:, c, o + 1:o + 513], scalar=ck(1, c), in1=tmp[:], op0=ALU.mult, op1=ALU.add),
                            rd + (("nt", tb),), (("nt", tb),))
                        dve(lambda e, tmp=tmp, c=c, o=o: e.scalar_tensor_tensor(out=tmp[:], in0=vT[:, c, o:o + 512], scalar=ck(0, c), in1=tmp[:], op0=ALU.mult, op1=ALU.add),
                            rd + (("nt", tb),), (("nt", tb),))
                        dst = cg[c4 % 2][:, ci, o:o + 512]
                        dve(lambda e, dst=dst, tmp=tmp, ba=ba: e.tensor_tensor(out=dst, in0=ps[ba][:], in1=tmp[:], op=ALU.mult),
                            (PS(ba), ("nt", tb)), (("cg", c4 % 2, ci, tt),))
            task([(KC, 512, 0, 512, wsrc(win, 0, KC, c4 * 512, 512))], fn)

            def fn2(s, c4=c4):
                wv = wview(s, 4, 2048)
                for nci in range(KC):
                    for tt in range(2):
                        b = 6 + dn_rot[0] % 2
                        dn_rot[0] += 1
                        for k in range(4):
                            mm(ps[b][:], wv[:, k, nci * 128:(nci + 1) * 128], cg[c4 % 2][:, k, tt * 512:(tt + 1) * 512],
                               k == 0, k == 3, (("w", s, 0), ("w", s, 1), ("cg", c4 % 2, k, tt)), (PS(b),))
                        xs = xT[:, nci, tt * 512:(tt + 1) * 512]
                        dve(lambda e, xs=xs, b=b, nci=nci: e.scalar_tensor_tensor(out=xs, in0=ps[b][:], scalar=ABc(2, nci), in1=xs, op0=ALU.mult, op1=ALU.add),
                            (PS(b), ("x", nci, tt), ("AB", 2)), (("x", nci, tt),))
            task([(4, 2048, 0, 2048, wsrc(wout, c4 * 512, 4, 0, 2048))], fn2)


    def gla_mixer(i):
        j = i // 2
        win, wout, wgk_d, gng_d = W[("gin", i)], W[("gout", i)], W[("wgk", i)], W[("gng", i)]
        qT = carve(0, 2048, BF16, "p (k t) -> p k t", t=T)
        kdec = carve(2048, 2048, BF16, "p (a n) -> p a n", n=256)
        vbf = carve(4096, 4096, BF16, "p (a n) -> p a n", n=512)
        sg = carve(8192, 4096, BF16, "p (a n) -> p a n", n=512)
        S = carve(12288, 2048, F32, "p (k n) -> p k n", n=512)
        S2 = carve(12288, 2048, F32)
        Sbf = carve(14336, 1024, BF16, "p (k n) -> p k n", n=512)
        oT = carve(15360, 4096, BF16, "p (k t) -> p k t", t=T)
        og = carve(19456, 1024, F32)
        spb = [carve(20480 + q * 512, 512, F32) for q in range(2)]
        etb = [carve(21504 + q * 512, 512, F32) for q in range(2)]
        gkT = arena[0:32, 22528:24576].bitcast(F32)
        dec = carve(24576, 64, F32)
        ngb, wgk = nt[0], nt[1]

        def setup(_):
            dve(lambda e: e.memset(gkT, 1.0), (), ("gkT",))
            sp_dma(ngb[:], gng_d, (), (("nt", 0),), f"gng{i}")
        task(None, setup)

        def t0(s):
            wv = wview(s, KC, 16)
            for tt in range(2):
                for kc in range(KC):
                    mm(ps[7][0:16, :], wv[:, kc, 0:16], hT[:, kc, tt * 512:(tt + 1) * 512], kc == 0, kc == KC - 1,
                       (("w", s, 0), ("w", s, 1), ("h", kc, tt)), (PS(7),))
                act(gkT[0:16, tt * 512:(tt + 1) * 512], ps[7][0:16, :], AF.Copy, (PS(7),), ("gkT",))
        task([(KC, 16, 0, 16, wsrc(win, 0, KC, 6144, 16))], t0)

        for h in range(4):
            def tq(s, h=h):
                wv = wview(s, KC, 256)
                for dkc in range(2):
                    for tt in range(2):
                        b = 4 + (2 * dkc + tt) % 4
                        for kc in range(KC):
                            mm(ps[b][:], wv[:, kc, dkc * 128:(dkc + 1) * 128], hT[:, kc, tt * 512:(tt + 1) * 512], kc == 0, kc == KC - 1,
                               (("w", s, 0), ("w", s, 1), ("h", kc, tt)), (PS(b),))
                        act(qT[:, dkc, tt * 512:(tt + 1) * 512], ps[b][:], AF.Copy, (PS(b),), (("q", dkc, tt),), scale=1.0 / 16.0)
            task([(KC, 256, 0, 256, wsrc(win, 0, KC, h * 256, 256))], tq)

            def tk(s, h=h):
                wv = wview(s, KC, 256)
                sp_dma(wgk[0:17, 0:256], wgk_d[:, h * 256:(h + 1) * 256], (), (("nt", 1),), f"wgk{i}")
                for tile in range(8):
                    bk, bz, bD = tile % 2, 2 + tile % 2, 4 + tile % 2
                    tk_ = slice(tile * 128, (tile + 1) * 128)
                    for kc in range(KC):
                        mm(ps[bk][:, 0:256], hT[:, kc, tk_], wv[:, kc, 0:256], kc == 0, kc == KC - 1,
                           (("w", s, 0), ("w", s, 1), ("h", kc, tile // 4)), (PS(bk),))
                    mm(ps[bz][:, 0:256], gkT[0:17, tk_], wgk[0:17, 0:256], True, True, ("gkT", ("nt", 1)), (PS(bz),))
                    et, sp_ = etb[tile % 2], spb[tile % 2]
                    act(et, ps[bz][:, 0:256], AF.Exp, (PS(bz),), (("et", tile % 2),), scale=-1.0)
                    act(sp_, et, AF.Ln, (("et", tile % 2),), (("sp", tile % 2),), bias=1.0)
                    mm(ps[bD][:, 0:256], u2[:], sp_, True, True, ("u2", ("sp", tile % 2)), (PS(bD),))
                    for dkc in range(2):
                        col = dkc * 16 + tile * 2
                        mm(ps[6][:, col:col + 2], sp_[:, dkc * 128:(dkc + 1) * 128], ind2[:], True, True,
                           ("ind2", ("sp", tile % 2)), (PS(6),))
                    act(et, ps[bD][:, 0:256], AF.Exp, (PS(bD),), (("et", tile % 2),))
                    dve(lambda e, tile=tile, bk=bk, et=et: e.tensor_tensor(out=kdec[:, tile, :], in0=ps[bk][:, 0:256], in1=et, op=ALU.mult),
                        (PS(bk), ("et", tile % 2)), (("kdec", tile),))
                act(dec, ps[6][:, 0:32], AF.Exp, (PS(6),), ("dec",))
            task([(KC, 256, 0, 256, wsrc(win, 0, KC, 1024 + h * 256, 256))], tk)

            def tv(s, h=h):
                wv = wview(s, KC, 512)
                for tile in range(8):
                    b = tile % 2
                    for kc in range(KC):
                        mm(ps[b][:], hT[:, kc, tile * 128:(tile + 1) * 128], wv[:, kc, :], kc == 0, kc == KC - 1,
                           (("w", s, 0), ("w", s, 1), ("h", kc, tile // 4)), (PS(b),))
                    act(vbf[:, tile, :], ps[b][:], AF.Copy, (PS(b),), (("gv", tile),))
            task([(KC, 512, 0, 512, wsrc(win, 0, KC, 2048 + h * 512, 512))], tv)

            def tg(s, h=h):
                wv = wview(s, KC, 512)
                for tile in range(8):
                    b = 2 + tile % 2
                    for kc in range(KC):
                        mm(ps[b][:], hT[:, kc, tile * 128:(tile + 1) * 128], wv[:, kc, :], kc == 0, kc == KC - 1,
                           (("w", s, 0), ("w", s, 1), ("h", kc, tile // 4)), (PS(b),))
                    act(sg[:, tile, :], ps[b][:], AF.Silu, (PS(b),), (("sg", tile),))
            task([(KC, 512, 0, 512, wsrc(win, 0, KC, 4096 + h * 512, 512))], tg)

            def kv_step(c):
                tile, half = c // 2, c % 2
                rows = slice(half * 64, half * 64 + 64)
                for dkc in range(2):
                    b = (c % 2) * 2 + dkc
                    mm(ps[b][:], kdec[rows, tile, dkc * 128:(dkc + 1) * 128], vbf[rows, tile, :], True, True,
                       (("kdec", tile), ("gv", tile)), (PS(b),))
                    dve(lambda e, dkc=dkc, b=b, c=c: e.scalar_tensor_tensor(out=S[:, dkc, :], in0=S[:, dkc, :], scalar=dec[:, dkc * 16 + c:dkc * 16 + c + 1],
                                                                            in1=ps[b][:], op0=ALU.mult, op1=ALU.add),
                        (PS(b), ("S", dkc), "dec"), (("S", dkc),))

            def scan1(_, h=h):
                dve(lambda e: e.memset(S2, 0.0), (), (("S", 0), ("S", 1)))
                for c in range(16):
                    kv_step(c)
                sp_dma(cc_s_in.ap(), S2, (("S", 0), ("S", 1)), ("cc_s_in",), f"sx{i}a")
                P.add("pool", (lambda e: e.collective_compute("AllGather", ALU.bypass, replica_groups=PAIRS,
                                                              ins=[cc_s_in.ap().opt()], outs=[cc_s_out.ap().opt()])),
                      reads=("cc_s_in",), writes=("cc_s_out",), dsem=f"sx{i}b", inc=1)
                sp_dma(S2, cc_s_out.ap()[0:128, :], ("cc_s_out",), (("S", 0), ("S", 1)), f"sx{i}c")
                dve(lambda e: e.tensor_scalar(out=S2, in0=S2, scalar1=flag[:, 0:1], scalar2=None, op0=ALU.mult),
                    (("S", 0), ("S", 1), "flag"), (("S", 0), ("S", 1)))
            task(None, scan1)

            def scan2(_, h=h):
                for c in range(16):
                    tile, half = c // 2, c % 2
                    rows = slice(half * 64, half * 64 + 64)
                    kv_step(c)
                    bo = 4 + c % 2
                    for dkc in range(2):
                        act(Sbf[:, dkc, :], S[:, dkc, :], AF.Copy, (("S", dkc),), (("Sbf", dkc),))
                        mm(ps[bo][:], qT[:, dkc, tile * 128:(tile + 1) * 128], Sbf[:, dkc, :], dkc == 0, dkc == 1,
                           (("q", dkc, tile // 4), ("Sbf", dkc)), (PS(bo),))
                    ssc = rstd[rows, half:half + 1]
                    act(og[rows, :], ps[bo][rows, :], AF.Square, (PS(bo),), (("og", half),))
                    dve(lambda e, rows=rows, ssc=ssc: e.reduce_sum(out=ssc, in_=og[rows, :], axis=AX.X), (("og", half),), (("ss", half),))
                    act(ssc, ssc, AF.Sqrt, (("ss", half),), (("ss", half),), scale=1.0 / 512.0, bias=EPS)
                    dve(lambda e, ssc=ssc: e.reciprocal(out=ssc, in_=ssc), (("ss", half),), (("ss", half),))
                    dve(lambda e, rows=rows, ssc=ssc, bo=bo: e.scalar_tensor_tensor(out=og[rows, :], in0=ps[bo][rows, :], scalar=ssc, in1=ngb[rows, :],
                                                                                     op0=ALU.mult, op1=ALU.mult),
                        (PS(bo), ("ss", half), ("nt", 0)), (("og", half),))
                    dve(lambda e, rows=rows, tile=tile: e.tensor_tensor(out=og[rows, :], in0=og[rows, :], in1=sg[rows, tile, :], op=ALU.mult),
                        (("og", half), ("sg", tile)), (("og", half),))
                    if half == 1:
                        bt = 6 + tile % 2
                        for jx in range(4):
                            P.add("pe", (lambda e, bt=bt, jx=jx: e.transpose(ps[bt][:, jx * 128:(jx + 1) * 128], og[:, jx * 128:(jx + 1) * 128], ident[:])),
                                  reads=(("og", 0), ("og", 1), "ident"), writes=(PS(bt),))
                        act(oT[:, :, tile * 128:(tile + 1) * 128], ps[bt][:].rearrange("p (k t) -> p k t", t=128), AF.Copy,
                            (PS(bt),), (("oT", tile),))
            task(None, scan2)

            def tout(s, h=h):
                wv = wview(s, 4, 2048)
                for nci in range(KC):
                    for tt in range(2):
                        b = 2 + dn_rot[0] % 2
                        dn_rot[0] += 1
                        for k in range(4):
                            mm(ps[b][:], wv[:, k, nci * 128:(nci + 1) * 128], oT[:, k, tt * 512:(tt + 1) * 512], k == 0, k == 3,
                               (("w", s, 0), ("w", s, 1)) + tuple(("oT", tt * 4 + q) for q in range(4)), (PS(b),))
                        xs = xT[:, nci, tt * 512:(tt + 1) * 512]
                        dve(lambda e, xs=xs, b=b, nci=nci: e.scalar_tensor_tensor(out=xs, in0=ps[b][:], scalar=ABc(2, nci), in1=xs, op0=ALU.mult, op1=ALU.add),
                            (PS(b), ("x", nci, tt), ("AB", 2)), (("x", nci, tt),))
            task([(4, 2048, 0, 2048, wsrc(wout, h * 512, 4, 0, 2048))], tout)

    def fence(keys=()):
        dve(lambda e: e.memset(dummy[:], 0.0), (), ("EPOCH",))

    def moe(i):
        j = i // 2
        rt_d, mw13, mw2 = W[("rt", i)], W[("mw13", i)], W[("mw2", i)]
        flags_i = flags_all[j]
        fkey = ("flags", j)
        RB = 16384
        Rr = carve(RB, 256, F32, "p (k e) -> p k e", e=NE)
        RA = carve(RB + 256, 256, F32, "p (k e) -> p k e", e=NE)
        logT = carve(RB + 512, 1024, F32)
        L = carve(RB + 1536, 128, F32, "p (a e) -> p a e", e=NE)
        L2 = carve(RB + 1664, 128, F32, "p (a e) -> p a e", e=NE)
        E2 = carve(RB + 1792, 128, F32, "p (a e) -> p a e", e=NE)
        m1 = carve(RB + 1920, 16, F32)
        m2 = carve(RB + 1936, 16, F32)
        dd = carve(RB + 1952, 16, F32)
        g1 = carve(RB + 1968, 16, F32)
        g2 = carve(RB + 1984, 16, F32)
        cst = carve(RB + 2000, 2, F32)
        Wt = msm[:, 0:64].rearrange("p (a e) -> p a e", e=NE)
        Wtf = msm[:, 0:64]
        Mx = msm[:, 64:128]
        Mx3 = msm[:, 64:128].rearrange("p (a e) -> p a e", e=NE)
        t1 = msm[:, 128:192]
        rk = msm[:, 192:256]
        rkp = msm[:, 256:512].rearrange("p (q a e) -> p q a e", q=4, e=NE)
        h_tm = carve(0, 16384, BF16, "p (a f) -> p a f", f=D)
        SS = 512
        Sel = carve(16384, 4096, BF16, "p (a s) -> p a s", s=SS)
        SelT = [carve(20480 + st * 1024, 1024, BF16) for st in range(4)]
        hflat = hT[:].rearrange("p k t -> p (k t)")
        hsel = hflat[:, 0:8192].rearrange("p (k s) -> p k s", s=SS)
        ybf = hflat[:, 8192:16384].rearrange("p (a f) -> p a f", f=D)
        actm = ws[2][:, 0:6144].rearrange("p (c s) -> p c s", s=SS)
        iota = rstd[:, 0:512]
        hkeys = [("h", kc, tt) for kc in range(KC) for tt in range(2)]
        rkeys = ["Rr", "cst", "logT", "L", "dd", "g1", "g2"] + [("RA", kc) for kc in range(KC)] + \
                [(nm, a) for nm in ("m1", "m2", "L2", "E2") for a in range(8)]
        nkeys = []

        def setup(_):
            sp_dma(Rr, rt_d, (), ("Rr",), f"rt{i}")
            for kc in range(KC):
                dve(lambda e, kc=kc: e.tensor_scalar(out=RA[:, kc, :], in0=Rr[:, kc, :], scalar1=ABc(3, kc), scalar2=None, op0=ALU.mult),
                    ("Rr", ("AB", 3)), (("RA", kc),))
            for kc in range(KC):
                mm(ps[5][0:8, 0:1], Rr[:, kc, :], ABc(4, kc), kc == 0, kc == KC - 1, ("Rr", ("AB", 4)), (PS(5),))
            dve(lambda e: e.tensor_copy(out=cst[0:8, :], in_=ps[5][0:8, 0:1]), (PS(5),), ("cst",))
        task(None, setup)

        def extra(tt):
            for kc in range(KC):
                mm(ps[5][0:8, :], RA[:, kc, :], xT[:, kc, tt * 512:(tt + 1) * 512], kc == 0, kc == KC - 1,
                   (("RA", kc), ("x", kc, tt)), (PS(5),))
            dve(lambda e: e.tensor_tensor(out=logT[0:8, :], in0=ps[5][0:8, :], in1=rstd[0:8, :], op=ALU.mult), (PS(5), "rstd"), ("logT",))
            dve(lambda e: e.tensor_scalar(out=logT[0:8, :], in0=logT[0:8, :], scalar1=cst[0:8, 0:1], scalar2=None, op0=ALU.add),
                ("logT", "cst"), ("logT",))
            for q in range(4):
                tile = tt * 4 + q
                P.add("pe", (lambda e, tile=tile, q=q: e.transpose(ps[4][:, tile * 8:(tile + 1) * 8], logT[0:8, q * 128:(q + 1) * 128], ident[0:8, 0:8])),
                      reads=("logT", "ident"), writes=(PS(4),))
        norm_mod(1, extra)

        def gates(_):
            sp_dma(iota, iota_d, (), ("rstd",), f"iota{i}")
            dve(lambda e: e.tensor_copy(out=L, in_=ps[4][:, 0:64].rearrange("p (a e) -> p a e", e=NE)), (PS(4),), ("L",))
            for a in range(8):
                dve(lambda e, a=a: e.tensor_reduce(out=m1[:, a:a + 1], in_=L[:, a, :], axis=AX.X, op=ALU.max), ("L",), (("m1", a),))
                dve(lambda e, a=a: e.tensor_scalar(out=L2[:, a, :], in0=L[:, a, :], scalar1=m1[:, a:a + 1], scalar2=-1e30, op0=ALU.is_equal, op1=ALU.mult),
                    ("L", ("m1", a)), (("L2", a),))
                dve(lambda e, a=a: e.tensor_tensor(out=L2[:, a, :], in0=L2[:, a, :], in1=L[:, a, :], op=ALU.add), ("L", ("L2", a)), (("L2", a),))
                dve(lambda e, a=a: e.tensor_reduce(out=m2[:, a:a + 1], in_=L2[:, a, :], axis=AX.X, op=ALU.max), (("L2", a),), (("m2", a),))
            rm = tuple(("m1", a) for a in range(8)) + tuple(("m2", a) for a in range(8))
            dve(lambda e: e.tensor_tensor(out=dd[:, 0:8], in0=m1[:, 0:8], in1=m2[:, 0:8], op=ALU.subtract), rm, ("dd",))
            act(g1[:, 0:8], dd[:, 0:8], AF.Sigmoid, ("dd",), ("g1",))
            act(g2[:, 0:8], dd[:, 0:8], AF.Sigmoid, ("dd",), ("g2",), scale=-1.0)
            for a in range(8):
                dve(lambda e, a=a: e.tensor_scalar(out=Wt[:, a, :], in0=L[:, a, :], scalar1=m1[:, a:a + 1], scalar2=g1[:, a:a + 1], op0=ALU.is_equal, op1=ALU.mult),
                    ("L", ("m1", a), "g1"), (("Wt", a),))
                dve(lambda e, a=a: e.tensor_scalar(out=E2[:, a, :], in0=L2[:, a, :], scalar1=m2[:, a:a + 1], scalar2=g2[:, a:a + 1], op0=ALU.is_equal, op1=ALU.mult),
                    (("L2", a), ("m2", a), "g2"), (("E2", a),))
                dve(lambda e, a=a: e.tensor_tensor(out=Wt[:, a, :], in0=Wt[:, a, :], in1=E2[:, a, :], op=ALU.add), (("Wt", a), ("E2", a)), (("Wt", a),))
            wtk = tuple(("Wt", a) for a in range(8))
            dve(lambda e: e.tensor_single_scalar(out=Mx, in_=Wtf, scalar=0.0, op=ALU.is_gt), wtk, ("Mx",))
            for a in range(8):
                for a2 in range(a):
                    mm(ps[3][:, a * 8:(a + 1) * 8], ones[:], Mx3[:, a2, :], a2 == 0, False, ("Mx", "ones"), (PS(3),))
                mm(ps[3][:, a * 8:(a + 1) * 8], lt[:], Mx3[:, a, :], a == 0, True, ("Mx", "lt"), (PS(3),))
            for a in range(8):
                mm(ps[2][0:1, 0:8], ones[:, 0:1], Mx3[:, a, :], a == 0, a == 7, ("Mx", "ones"), (PS(2),))
            dve(lambda e: e.tensor_copy(out=cntf[:], in_=ps[2][0:1, 0:8]), (PS(2),), ("cntf",))
            cf = msm[0:1, 384:416]
            for q_, thr in enumerate((0.5, 256.5, 384.5, 512.5)):
                dve(lambda e, q_=q_, thr=thr: e.tensor_single_scalar(out=cf[:, q_ * 8:(q_ + 1) * 8], in_=cntf[:], scalar=thr, op=ALU.is_gt),
                    ("cntf",), (("cf", q_),))
            dve(lambda e: e.tensor_tensor(out=flags_i[0:1, 0:8], in0=cf[:, 0:8], in1=cf[:, 8:16], op=ALU.subtract), (("cf", 0), ("cf", 1)), (fkey,))
            dve(lambda e: e.tensor_tensor(out=flags_i[0:1, 8:16], in0=cf[:, 8:16], in1=cf[:, 16:24], op=ALU.subtract), (("cf", 1), ("cf", 2)), (fkey,))
            dve(lambda e: e.tensor_copy(out=flags_i[0:1, 16:24], in_=cf[:, 16:24]), (("cf", 2),), (fkey,))
            dve(lambda e: e.tensor_copy(out=flags_i[0:1, 24:32], in_=cf[:, 24:32]), (("cf", 3),), (fkey,))
            dve(lambda e: e.tensor_scalar(out=t1, in0=Mx, scalar1=-1.0, scalar2=1e9, op0=ALU.add, op1=ALU.mult), ("Mx",), ("t1",))
            dve(lambda e: e.tensor_tensor(out=rk, in0=ps[3][:, 0:64], in1=Mx, op=ALU.mult), (PS(3), "Mx"), ("rk",))
            dve(lambda e: e.tensor_tensor(out=rk, in0=rk, in1=t1, op=ALU.add), ("rk", "t1"), ("rk",))
            dve(lambda e: e.tensor_copy(out=wtb[:], in_=Wtf), wtk, ("wtb",))
            for q in range(2):
                dve(lambda e, q=q: e.tensor_scalar(out=msm[:, 256 + q * 64:256 + (q + 1) * 64], in0=rk, scalar1=-512.0 * q, scalar2=None, op0=ALU.add),
                    ("rk",), (("rkp", q),))
            n_ = 0
            for a in range(8):
                for half in range(2):
                    b = n_ % 4
                    psb = ps[b][:].bitcast(BF16)
                    for q in range(8):
                        kc = half * 8 + q
                        P.add("pe", (lambda e, psb=psb, q=q, kc=kc, a=a: e.transpose(psb[:, q * 128:(q + 1) * 128], hT[:, kc, a * 128:(a + 1) * 128], identb[:])),
                              reads=(("h", kc, a // 4), "identb"), writes=(PS(b),))
                    dst = h_tm[:, a, half * 1024:(half + 1) * 1024]
                    if n_ % 2 == 0:
                        act(dst, psb, AF.Copy, (PS(b),), (("htm", a),))
                    else:
                        dve(lambda e, dst=dst, psb=psb: e.tensor_copy(out=dst, in_=psb), (PS(b),), (("htm", a),))
                    n_ += 1
            fence(hkeys + rkeys + nkeys)
        task(None, gates)

        rot = [0]
        wtb3 = wtb[:].rearrange("p (a e) -> p a e", e=NE)
        for e_ in range(NE):
            for (S_, fi_, q) in ((256, 0, 0), (384, 1, 0), (512, 2, 0), (512, 3, 1)):
                NST = S_ // 128
                tasks.append(("rb", flags_i[0:1, fi_ * 8 + e_:fi_ * 8 + e_ + 1], fkey))

                def pre(_, e_=e_, q=q, S_=S_, NST=NST):
                    for a in range(8):
                        dve(lambda e, a=a: e.tensor_scalar(out=Sel[:, a, 0:S_], in0=iota[:, 0:S_], scalar1=rkp[:, q, a, e_:e_ + 1], scalar2=None, op0=ALU.is_equal),
                            ("rstd", ("rkp", q)), (("Sel", a),))
                    for st in range(NST):
                        for a in range(8):
                            mm(ps[7][:, st:st + 1], Sel[:, a, st * 128:(st + 1) * 128], wtb3[:, a, e_:e_ + 1], a == 0, a == 7,
                               (("Sel", a), "wtb"), (PS(7),))
                    dve(lambda e: e.tensor_copy(out=gslot[:, 0:NST], in_=ps[7][:, 0:NST]), (PS(7),), ("gslot",))
                    for kc in range(KC):
                        b = kc % 4
                        for a in range(8):
                            mm(ps[b][:, 0:S_], h_tm[:, a, kc * 128:(kc + 1) * 128], Sel[:, a, 0:S_], a == 0, a == 7,
                               (("htm", a), ("Sel", a)), (PS(b),))
                        dst = hsel[:, kc, 0:S_]
                        if kc % 2 == 0:
                            act(dst, ps[b][:, 0:S_], AF.Copy, (PS(b),), (("hsel", kc),))
                        else:
                            dve(lambda e, dst=dst, b=b: e.tensor_copy(out=dst, in_=ps[b][:, 0:S_]), (PS(b),), (("hsel", kc),))
                    for st in range(NST):
                        b = 4 + st % 2
                        psb = ps[b][:].bitcast(BF16)
                        for a in range(8):
                            P.add("pe", (lambda e, psb=psb, a=a, st=st: e.transpose(psb[:, a * 128:(a + 1) * 128], Sel[:, a, st * 128:(st + 1) * 128], identb[:])),
                                  reads=(("Sel", a), "identb"), writes=(PS(b),))
                        if st % 2 == 0:
                            act(SelT[st], psb, AF.Copy, (PS(b),), (("SelT", st),))
                        else:
                            dve(lambda e, psb=psb, st=st: e.tensor_copy(out=SelT[st], in_=psb), (PS(b),), (("SelT", st),))
                task(None, pre)

                w13, w2 = mw13[e_], mw2[e_]
                for (c0, n) in SECTIONS:
                    for blk in range(n // 2):
                        c = c0 + 2 * blk

                        def fn(s, c=c, c0=c0, S_=S_):
                            wv = wview(s, KC, 512)
                            for ci in range(2):
                                r = rot[0] % 3
                                rot[0] += 1
                                ba, bb = 2 * r, 2 * r + 1
                                for kc in range(KC):
                                    mm(ps[ba][:, 0:S_], wv[:, kc, ci * 128:(ci + 1) * 128], hsel[:, kc, 0:S_], kc == 0, kc == KC - 1,
                                       (("w", s, 0), ("w", s, 1), ("hsel", kc)), (PS(ba),))
                                    mm(ps[bb][:, 0:S_], wv[:, kc, 256 + ci * 128:256 + (ci + 1) * 128], hsel[:, kc, 0:S_], kc == 0, kc == KC - 1,
                                       (("w", s, 0), ("w", s, 1), ("hsel", kc)), (PS(bb),))
                                sb_ = r % 2
                                act(sa[sb_][:, 0:S_], ps[ba][:, 0:S_], AF.Silu, (PS(ba),), (("sa", sb_),))
                                dst = actm[:, c + ci - c0, 0:S_]
                                dve(lambda e, dst=dst, sb_=sb_, bb=bb: e.tensor_tensor(out=dst, in0=ps[bb][:, 0:S_], in1=sa[sb_][:, 0:S_], op=ALU.mult),
                                    (PS(bb), ("sa", sb_)), (("actm", c + ci - c0),))
                        task([(KC, 512, 0, 256, wsrc(w13, 0, KC, c * 128, 256)),
                              (KC, 512, 256, 256, wsrc(w13, 0, KC, DFF + c * 128, 256))], fn)
                    for nb in range(4):
                        def fn2(s, nb=nb, n=n, NST=NST):
                            wv = wview(s, n, 512)
                            for st in range(NST):
                                b = 6 + dn_rot[0] % 2
                                dn_rot[0] += 1
                                for k in range(n):
                                    mm(ps[b][:], actm[:, k, st * 128:(st + 1) * 128], wv[:, k, :], k == 0, k == n - 1,
                                       (("w", s, 0), ("w", s, 1), ("actm", k)), (PS(b),))
                                dst = ybf[:, st, nb * 512:(nb + 1) * 512]
                                if st % 2 == 0:
                                    act(dst, ps[b][:], AF.Copy, (PS(b), "gslot"), (("ybf", st, nb),), scale=gslot[:, st:st + 1])
                                else:
                                    dve(lambda e, dst=dst, b=b, st=st: e.tensor_scalar(out=dst, in0=ps[b][:], scalar1=gslot[:, st:st + 1], scalar2=None, op0=ALU.mult),
                                        (PS(b), "gslot"), (("ybf", st, nb),))
                        task([(n, 512, 0, 512, wsrc(w2, c0 * 128, n, nb * 512, 512))], fn2)

                    def comb(_, NST=NST):
                        for c in range(KC):
                            for tt in range(2):
                                b = rot[0] % 3 * 2 + (rot[0] // 3) % 2
                                rot[0] += 1
                                for st in range(NST):
                                    mm(ps[b][:], ybf[:, st, c * 128:(c + 1) * 128], SelT[st][:, tt * 512:(tt + 1) * 512], st == 0, st == NST - 1,
                                       (("ybf", st, c // 4), ("SelT", st)), (PS(b),))
                                xs = xT[:, c, tt * 512:(tt + 1) * 512]
                                dve(lambda e, xs=xs, b=b, c=c: e.scalar_tensor_tensor(out=xs, in0=ps[b][:], scalar=ABc(5, c), in1=xs, op0=ALU.mult, op1=ALU.add),
                                    (PS(b), ("x", c, tt), ("AB", 5)), (("x", c, tt),))
                    task(None, comb)
                tasks.append(("re",))

        def fin(_):
            fence(hkeys + rkeys + nkeys)
        task(None, fin)

    def store(_):
        for tt in range(2):
            if final_norm:
                norm_stats(tt, 6)
            for kc in range(KC):
                b = kc % 2
                src = xT[:, kc, tt * 512:(tt + 1) * 512]
                if final_norm:
                    dve(lambda e, b=b, kc=kc, src=src: e.scalar_tensor_tensor(out=nt[b][:], in0=src, scalar=fgT[:, kc:kc + 1], in1=rstd[:],
                                                                              op0=ALU.mult, op1=ALU.mult),
                        (("x", kc, tt), "rstd", "fgT"), (("nt", b),))
                    src = nt[b][:]
                    rd = (("nt", b),)
                else:
                    rd = (("x", kc, tt),)
                sp_dma(out_d[kc * 128:(kc + 1) * 128, tt * 512:(tt + 1) * 512], src, rd, (("out", kc, tt),), f"o{b}")
        P.add("sp", (lambda e: e.nop()), reads=tuple(("out", kc, tt) for kc in range(KC) for tt in range(2)), writes=())

    for i in layers:
        ada_layer(i)
    for kind, i in plan:
        task(None, lambda _: fence())
        prep_AB(i)
        if kind == "mixer":
            norm_mod(0)
            if i % 2 == 0:
                conv_mixer(i)
            else:
                gla_mixer(i)
        else:
            if i % 2 == 0:
                norm_mod(1)
                ffn(W[("w13", i)], W[("w2", i)], 5)
            else:
                moe(i)
    task(None, store)
    run_tasks()
    P.emit()
    es.close()
    return nc, P


def _consts():
    ident = np.eye(128, dtype=np.float32)
    ones = np.ones((128, 128), np.float32)
    s = np.arange(128)[:, None]
    t = np.arange(128)[None, :]
    u2 = (((s // 64) == (t // 64)) & (s > t)).astype(np.float32) * (-1.0 / 16.0)
    ind2 = ((s // 64) == np.arange(2)[None, :]).astype(np.float32) * (-1.0 / 16.0)
    lt = (s < t).astype(np.float32)
    iota = np.ascontiguousarray(np.broadcast_to(np.arange(512, dtype=np.float32)[None, :], (128, 512)))
    return ident, ones, u2, ind2, lt, iota


def make_in_maps(plan, xs, inputs):
    ident, ones, u2, ind2, lt, iota = _consts()
    layers = sorted({i for _, i in plan})
    c = inputs["c"]
    ada_b = np.ascontiguousarray(inputs["ada_b"].reshape(4, 96, 128).transpose(0, 2, 1))
    ng = np.ascontiguousarray(inputs["norm_g"].reshape(4, 2, 16, 128).transpose(3, 0, 1, 2).reshape(128, 128))
    fg = np.ascontiguousarray(inputs["final_g"].reshape(16, 128).T)
    ck = np.ascontiguousarray(inputs["conv_k"].reshape(2, 3, 16, 128).transpose(3, 0, 1, 2).reshape(128, 96))
    shared = {"ada_bT": ada_b, "norm_gT": ng, "final_gT": fg, "conv_kT": ck, "ident": ident, "ones": ones, "u2": u2, "ind2": ind2, "lt": lt, "iota": iota}
    for i in layers:
        shared[f"ada_w{i}"] = inputs["ada_w"][i]
    for kind, i in plan:
        j = i // 2
        if kind == "mixer" and i % 2 == 0:
            shared[f"conv_w_in{j}"] = inputs["conv_w_in"][j]
            shared[f"conv_w_out{j}"] = inputs["conv_w_out"][j]
        if kind == "mixer" and i % 2 == 1:
            shared[f"gla_w_in{j}"] = inputs["gla_w_in"][j]
            shared[f"gla_w_out{j}"] = inputs["gla_w_out"][j]
            shared[f"gla_wgk{j}"] = np.concatenate([inputs["gla_w_gk"][j], inputs["gla_b_gk"][j][None, :]], 0)
            shared[f"gla_ng{j}"] = np.ascontiguousarray(np.broadcast_to(inputs["gla_norm_g"][j][None, :], (128, 512)))
        if kind == "ffn" and i % 2 == 0:
            shared[f"ffn_w13_{j}"] = inputs["ffn_w13"][j]
            shared[f"ffn_w2_{j}"] = inputs["ffn_w2"][j]
        if kind == "ffn" and i % 2 == 1:
            shared[f"moe_router{j}"] = np.ascontiguousarray(inputs["moe_router"][j].reshape(16, 128, 8).transpose(1, 0, 2))
            shared[f"moe_w13_{j}"] = inputs["moe_w13"][j]
            shared[f"moe_w2_{j}"] = inputs["moe_w2"][j]
    in_maps = []
    for r in range(8):
        b, hf = r // 2, r % 2
        m = dict(shared)
        m["xT"] = np.ascontiguousarray(xs[r].T)
        m["cT"] = np.ascontiguousarray(c[b].reshape(16, 128).T)
        m["flag"] = np.full((128, 1), float(hf), np.float32)
        in_maps.append(m)
    return in_maps


def run_plan(plan, xs, inputs, final_norm, trace=False):
    nc, P = build_program(plan, final_norm)
    in_maps = make_in_maps(plan, xs, inputs)
    res = run_bass_kernel_spmd(nc, in_maps, core_ids=list(range(8)), trace=trace)
    outs = [np.ascontiguousarray(res.results[r]["outT"].T) for r in range(8)]
    return outs, res


def shard_x(x):
    return [np.ascontiguousarray(x[r // 2, (r % 2) * T:(r % 2 + 1) * T, :]) for r in range(8)]


def unshard_x(outs):
    full = np.zeros((4, 2 * T, D), np.float32)
    for r in range(8):
        full[r // 2, (r % 2) * T:(r % 2 + 1) * T, :] = outs[r]
    return full


FULL_PLAN = [("mixer", 0), ("ffn", 0), ("mixer", 1), ("ffn", 1), ("mixer", 2), ("ffn", 2), ("mixer", 3), ("ffn", 3)]


def kernel(**inputs):
    inputs = {k: np.asarray(v) for k, v in inputs.items()}
    xs = shard_x(inputs["x"])
    outs, _ = run_plan(FULL_PLAN, xs, inputs, final_norm=True)
    return unshard_x(outs)
```
